# Optimizing a Trainium2 kernel written in Bass

```python
import math
import jax, jax.numpy as jnp
from jax import lax
import numpy as np

D_MODEL = 1024
BATCH = 16
SEQ = 2048
DEPTH = 2

GRID_W = 64
CTX_LEN = 256
CHUNK = 32
EPS = 1e-6
LB_FLOOR = 1e-30

MIX_WIDTH = D_MODEL
GLA_HEADS = 4
GLA_WIDTH = MIX_WIDTH // 2
GLA_DV = GLA_WIDTH // GLA_HEADS
GLA_DK = GLA_DV // 2
GLA_QK = GLA_HEADS * GLA_DK
GLA_GATE_RANK = 16
GLA_GATE_NORM = 16.0
HGRN_HEADS = 4
HGRN_WIDTH = MIX_WIDTH - GLA_WIDTH
HGRN_DV = HGRN_WIDTH // HGRN_HEADS
HGRN_DK = 128
HGRN_QK = HGRN_HEADS * HGRN_DK
IN_SPLITS = (GLA_QK, GLA_QK, GLA_WIDTH, GLA_WIDTH, GLA_GATE_RANK, GLA_GATE_RANK,
             HGRN_QK, HGRN_QK, HGRN_QK, HGRN_WIDTH, HGRN_WIDTH)
IN_WIDTH = sum(IN_SPLITS)
D_FF = 256 * math.ceil(8 * D_MODEL / 3 / 256)
N_EXPERTS = 8
TOP_K = 2
D_EXPERT = 7 * D_MODEL // 2
N_DENSE = (DEPTH + 1) // 2
N_MOE = DEPTH // 2

kernel_name = "hybrid_gla_hgrn2_moe_diffusion_block"


def rmsnorm(x, g):
    xf = x.astype(jnp.float32)
    y = xf * lax.rsqrt(jnp.mean(xf * xf, axis=-1, keepdims=True) + EPS)
    return (y * g.astype(jnp.float32)).astype(x.dtype)


def ada_rmsnorm(x, g, shift, scale):
    return rmsnorm(x, g) * (1 + scale) + shift


def to_heads(t, n_heads):
    b, l, _ = t.shape
    return t.reshape(b, l, n_heads, -1).transpose(0, 2, 1, 3)


def merge_heads(t):
    b, h, l, d = t.shape
    return t.transpose(0, 2, 1, 3).reshape(b, l, h * d)


def to_colmajor(t, rows):
    b, l, ch = t.shape
    return t.reshape(b, rows, GRID_W, ch).transpose(0, 2, 1, 3).reshape(b, l, ch)


def from_colmajor(t, rows):
    b, l, ch = t.shape
    return t.reshape(b, GRID_W, rows, ch).transpose(0, 2, 1, 3).reshape(b, l, ch)


def chunk_gla(q, k, v, log_f, s0):
    b_, h_, l_, _ = q.shape
    dv = v.shape[-1]
    n = l_ // CHUNK

    def blocks(t):
        return jnp.moveaxis(t.astype(jnp.float32).reshape(b_, h_, n, CHUNK, t.shape[-1]), 2, 0)

    lower_tri = jnp.tril(jnp.ones((CHUNK, CHUNK), dtype=bool))[:, :, None]

    def step(state, blk):
        qc, kc, vc, gc = blk
        b = jnp.cumsum(gc, axis=2)
        b_last = b[:, :, -1, :]
        o_inter = jnp.einsum('bhcd,bhde->bhce', qc * jnp.exp(b), state)
        diff = b[:, :, :, None, :] - b[:, :, None, :, :]
        decay = jnp.where(lower_tri, jnp.exp(jnp.where(lower_tri, diff, 0.0)), 0.0)
        scores = jnp.einsum('bhid,bhjd,bhijd->bhij', qc, kc, decay)
        o = o_inter + jnp.einsum('bhij,bhje->bhie', scores, vc)
        k_dec = kc * jnp.exp(b_last[:, :, None, :] - b)
        new_state = state * jnp.exp(b_last)[..., None] + jnp.einsum('bhcd,bhce->bhde', k_dec, vc)
        return new_state, o

    final, o = lax.scan(step, s0, (blocks(q), blocks(k), blocks(v), blocks(log_f)))
    o = jnp.moveaxis(o, 0, 2).reshape(b_, h_, l_, dv)
    return o, final


def bidir_recurrence(ctx_in, lat_in, n_heads, need_ctx):
    qc, kfc, kbc, vc, gfc, gbc = [to_heads(t, n_heads) for t in ctx_in]
    ql, kfl, kbl, vl, gfl, gbl = [to_heads(t, n_heads) for t in lat_in]
    flip = lambda t: jnp.flip(t, axis=2)
    b_, h_, _, dk = qc.shape
    s0 = jnp.zeros((b_, h_, dk, vc.shape[-1]), jnp.float32)
    o_cf, s_cf = chunk_gla(qc, kfc, vc, gfc, s0)
    o_cb, s_cb = chunk_gla(flip(qc), flip(kbc), flip(vc), flip(gbc), s0)
    o_lf, _ = chunk_gla(ql, kfl, vl, gfl, s_cf)
    o_lb, _ = chunk_gla(flip(ql), flip(kbl), flip(vl), flip(gbl), s_cb)
    o_lat = o_lf + flip(o_lb)
    o_ctx = (o_cf + flip(o_cb)) if need_ctx else None
    return o_ctx, o_lat


def hgrn_forget(f_pre, lb):
    f_pre = f_pre.astype(jnp.float32)
    log_lb = jnp.log(jnp.maximum(lb, LB_FLOOR))
    log_f = jnp.logaddexp(log_lb, jnp.log1p(-lb) + jax.nn.log_sigmoid(f_pre))
    k = (1.0 - lb) * jax.nn.sigmoid(-f_pre)
    return k, log_f


def project(h, w_in, gate_w2, gate_b, lb):
    p = h @ w_in
    offsets = np.cumsum(np.array(IN_SPLITS))[:-1].tolist()
    gq, gk, gv, gg, grf, grb, hq, hff, hfb, hv, hg = jnp.split(p, offsets, axis=-1)
    gla_q = gq * (GLA_DK ** -0.5)
    gla_lf = jax.nn.log_sigmoid((grf @ gate_w2[0] + gate_b[0]).astype(jnp.float32)) / GLA_GATE_NORM
    gla_lb = jax.nn.log_sigmoid((grb @ gate_w2[1] + gate_b[1]).astype(jnp.float32)) / GLA_GATE_NORM
    hq = jax.nn.silu(hq)
    hk_f, hlf_f = hgrn_forget(hff, lb[0])
    hk_b, hlf_b = hgrn_forget(hfb, lb[1])
    gla = (gla_q, gk, gk, gv, gla_lf, gla_lb)
    hgrn = (hq, hk_f, hk_b, hv, hlf_f, hlf_b)
    return gla, gg, hgrn, hg


def head_out(o, norm_g, dtype):
    return merge_heads(rmsnorm(o, norm_g).astype(dtype))


def token_mix(h_ctx, h_lat, w_in, gate_w2, gate_b, gla_g, hgrn_g, lb, w_out, need_ctx):
    rows = h_lat.shape[1] // GRID_W
    gla_c, gate_gla_c, hgrn_c, gate_hgrn_c = project(h_ctx, w_in, gate_w2, gate_b, lb)
    gla_l, gate_gla_l, hgrn_l, gate_hgrn_l = project(h_lat, w_in, gate_w2, gate_b, lb)
    o_gla_c, o_gla_l = bidir_recurrence(gla_c, gla_l, GLA_HEADS, need_ctx)
    hgrn_l_cm = tuple(to_colmajor(t, rows) for t in hgrn_l)
    o_hg_c, o_hg_l = bidir_recurrence(hgrn_c, hgrn_l_cm, HGRN_HEADS, need_ctx)
    dt = h_lat.dtype
    mix_l = jnp.concatenate([
        head_out(o_gla_l, gla_g, dt) * jax.nn.silu(gate_gla_l),
        from_colmajor(head_out(o_hg_l, hgrn_g, dt), rows) * jax.nn.silu(gate_hgrn_l)], axis=-1)
    y_lat = mix_l @ w_out
    y_ctx = None
    if need_ctx:
        mix_c = jnp.concatenate([
            head_out(o_gla_c, gla_g, dt) * jax.nn.silu(gate_gla_c),
            head_out(o_hg_c, hgrn_g, dt) * jax.nn.silu(gate_hgrn_c)], axis=-1)
        y_ctx = mix_c @ w_out
    return y_ctx, y_lat


def swiglu(h, w1, w3, w2):
    return (jax.nn.silu(h @ w1) * (h @ w3)) @ w2


def moe_swiglu(h, router, w1, w3, w2):
    b, l, d = h.shape
    t = h.reshape(b * l, d)
    logits = (t @ router).astype(jnp.float32)
    top_v, top_i = lax.top_k(logits, TOP_K)
    top_w = jax.nn.softmax(top_v, axis=-1)
    gates = jnp.sum(jax.nn.one_hot(top_i, N_EXPERTS, dtype=jnp.float32) * top_w[..., None], axis=1)
    gates = gates.astype(t.dtype)
    y = jnp.zeros_like(t)
    for e in range(N_EXPERTS):
        y = y + gates[:, e:e + 1] * swiglu(t, w1[e], w3[e], w2[e])
    return y.reshape(b, l, d)


def channel_mix(h, layer, ffn_w1, ffn_w3, ffn_w2, moe_router, moe_w1, moe_w3, moe_w2):
    i = layer // 2
    if layer % 2 == 0:
        return swiglu(h, ffn_w1[i], ffn_w3[i], ffn_w2[i])
    return moe_swiglu(h, moe_router[i], moe_w1[i], moe_w3[i], moe_w2[i])


def setup_inputs(seed: int = 0) -> dict:
    key = jax.random.key(seed)
    ks = jax.random.split(key, 24)
    nrm = lambda k, shape, s: jax.random.normal(k, shape, jnp.float32) * s
    D = D_MODEL
    return {
        "x": nrm(ks[0], (BATCH, SEQ, D), 1.0),
        "c": nrm(ks[1], (BATCH, D), 1.0),
        "ctx": nrm(ks[2], (BATCH, CTX_LEN, D), 1.0),
        "c_ctx": nrm(ks[3], (D,), 1.0),
        "w_mod": nrm(ks[4], (DEPTH, D, 6 * D), 0.5 * D ** -0.5),
        "b_mod": nrm(ks[5], (DEPTH, 6 * D), 0.01),
        "norm1_g": 1.0 + nrm(ks[6], (DEPTH, D), 0.05),
        "norm2_g": 1.0 + nrm(ks[7], (DEPTH, D), 0.05),
        "w_in": nrm(ks[8], (DEPTH, D, IN_WIDTH), D ** -0.5),
        "gla_gate_w2": nrm(ks[9], (DEPTH, 2, GLA_GATE_RANK, GLA_QK), GLA_GATE_RANK ** -0.5),
        "gla_gate_b": nrm(ks[10], (DEPTH, 2, GLA_QK), 0.1),
        "gla_norm_g": 1.0 + nrm(ks[11], (DEPTH, GLA_DV), 0.05),
        "hgrn_norm_g": 1.0 + nrm(ks[12], (DEPTH, HGRN_DV), 0.05),
        "hgrn_lb": 1.0 + nrm(ks[13], (DEPTH, 2, HGRN_QK), 0.1),
        "w_out": nrm(ks[14], (DEPTH, MIX_WIDTH, D), MIX_WIDTH ** -0.5),
        "ffn_w1": nrm(ks[15], (N_DENSE, D, D_FF), D ** -0.5),
        "ffn_w3": nrm(ks[16], (N_DENSE, D, D_FF), D ** -0.5),
        "ffn_w2": nrm(ks[17], (N_DENSE, D_FF, D), D_FF ** -0.5),
        "moe_router": nrm(ks[18], (N_MOE, D, N_EXPERTS), D ** -0.5),
        "moe_w1": nrm(ks[19], (N_MOE, N_EXPERTS, D, D_EXPERT), D ** -0.5),
        "moe_w3": nrm(ks[20], (N_MOE, N_EXPERTS, D, D_EXPERT), D ** -0.5),
        "moe_w2": nrm(ks[21], (N_MOE, N_EXPERTS, D_EXPERT, D), D_EXPERT ** -0.5),
        "final_norm_g": 1.0 + nrm(ks[22], (D,), 0.05),
    }


def reference(x, c, ctx, c_ctx, w_mod, b_mod, norm1_g, norm2_g, w_in, gla_gate_w2, gla_gate_b,
              gla_norm_g, hgrn_norm_g, hgrn_lb, w_out, ffn_w1, ffn_w3, ffn_w2, moe_router,
              moe_w1, moe_w3, moe_w2, final_norm_g):
    lb_soft = jax.nn.softmax(hgrn_lb.astype(jnp.float32), axis=0)
    lb_all = jnp.maximum(jnp.cumsum(lb_soft, axis=0) - lb_soft[0], 0.0)
    silu_c = jax.nn.silu(c)
    silu_cc = jax.nn.silu(c_ctx)
    for l in range(DEPTH):
        last = l == DEPTH - 1
        m_lat = jnp.split((silu_c @ w_mod[l] + b_mod[l])[:, None, :], 6, axis=-1)
        m_ctx = jnp.split(silu_cc @ w_mod[l] + b_mod[l], 6, axis=-1)
        h_lat = ada_rmsnorm(x, norm1_g[l], m_lat[0], m_lat[1])
        h_ctx = ada_rmsnorm(ctx, norm1_g[l], m_ctx[0], m_ctx[1])
        y_ctx, y_lat = token_mix(h_ctx, h_lat, w_in[l], gla_gate_w2[l], gla_gate_b[l],
                                 gla_norm_g[l], hgrn_norm_g[l], lb_all[l], w_out[l], not last)
        x = x + m_lat[2] * y_lat
        h_lat = ada_rmsnorm(x, norm2_g[l], m_lat[3], m_lat[4])
        x = x + m_lat[5] * channel_mix(h_lat, l, ffn_w1, ffn_w3, ffn_w2,
                                       moe_router, moe_w1, moe_w3, moe_w2)
        if not last:
            ctx = ctx + m_ctx[2] * y_ctx
            h_ctx = ada_rmsnorm(ctx, norm2_g[l], m_ctx[3], m_ctx[4])
            ctx = ctx + m_ctx[5] * channel_mix(h_ctx, l, ffn_w1, ffn_w3, ffn_w2,
                                               moe_router, moe_w1, moe_w3, moe_w2)
    return rmsnorm(x, final_norm_g)
```

```python
from contextlib import ExitStack
import numpy as np
import concourse.bass as bass
import concourse.mybir as mybir
from concourse.bass_utils import run_bass_kernel_spmd

F32 = mybir.dt.float32
BF16 = mybir.dt.bfloat16
AF = mybir.ActivationFunctionType
ALU = mybir.AluOpType

CE = ('pe', 'act', 'dve', 'pool', 'sp')
EPOCH = 12000
NLANE = 8


class Sched:
    def __init__(self):
        self.ops = {e: [] for e in CE}
        self.prod = {}
        self.last_w = {}
        self.readers = {}
        self.seen = {e: {} for e in CE}
        self.lane_rr = {'sp': 0, 'pool': 0}
        self.marked = set()
        self.barrier_dep = None

    def _deps(self, R, W):
        need = {}

        def add(p, i):
            if need.get(p, 0) < i:
                need[p] = i
        for k in R:
            lw = self.last_w.get(k)
            if lw:
                add(*lw)
        for k in W:
            lw = self.last_w.get(k)
            if lw:
                add(*lw)
            for p, i in self.readers.get(k, {}).items():
                add(p, i)
        if self.barrier_dep:
            for p, i in self.barrier_dep.items():
                add(p, i)
        return need

    def _note(self, pid, idx, R, W):
        for k in R:
            d = self.readers.setdefault(k, {})
            if d.get(pid, 0) < idx:
                d[pid] = idx
        for k in W:
            self.last_w[k] = (pid, idx)
            self.readers[k] = {}

    def _waits(self, stream, need, self_pid):
        waits = []
        for p, i in need.items():
            if p == self_pid and p == 'pe':
                continue
            if self.seen[stream].get(p, 0) >= i:
                continue
            self.seen[stream][p] = i
            waits.append((p, i))
            self.marked.add((p, i))
        return waits

    def op(self, eng, fn, R=(), W=()):
        need = self._deps(R, W)
        idx = self.prod.get(eng, 0) + 1
        self.prod[eng] = idx
        waits = self._waits(eng, need, eng)
        self.ops[eng].append(dict(fn=fn, waits=waits, pid=eng, idx=idx, dma=False))
        self._note(eng, idx, R, W)

    def dma(self, q, fn, R=(), W=()):
        lane = (q, self.lane_rr[q] % NLANE)
        self.lane_rr[q] += 1
        need = self._deps(R, W)
        idx = self.prod.get(lane, 0) + 1
        self.prod[lane] = idx
        if idx > 1:
            need[lane] = max(need.get(lane, 0), idx - 1)
        waits = self._waits(q, need, None)
        self.marked.add((lane, idx))
        self.ops[q].append(dict(fn=fn, waits=waits, pid=lane, idx=idx, dma=True))
        self._note(lane, idx, R, W)

    def barrier(self, fn_dve):
        need = dict(self.prod)
        self.barrier_dep = None
        idx = self.prod.get('dve', 0) + 1
        self.prod['dve'] = idx
        waits = self._waits('dve', need, 'dve')
        self.ops['dve'].append(dict(fn=fn_dve, waits=waits, pid='dve', idx=idx, dma=False))
        self.barrier_dep = {'dve': idx}

    def emit(self, nc, stack):
        ranks = {}
        by_p = {}
        for (p, i) in self.marked:
            by_p.setdefault(p, []).append(i)
        sems = {}
        for p, lst in by_p.items():
            lst.sort()
            for r, i in enumerate(lst):
                ranks[(p, i)] = r + 1
            nep = (len(lst) + EPOCH - 1) // EPOCH
            nm = p if isinstance(p, str) else f"{p[0]}{p[1]}"
            sems[p] = [stack.enter_context(nc.semaphore(f"s_{nm}_{k}")) for k in range(nep)]

        def semval(p, i):
            r = ranks[(p, i)]
            ep = (r - 1) // EPOCH
            v = r - ep * EPOCH
            return sems[p][ep], v * (1 if isinstance(p, str) else 16)

        def run(stream, eng):
            for o in self.ops[stream]:
                for (p, i) in o['waits']:
                    s, v = semval(p, i)
                    eng.wait_ge(s, v)
                if o['fn'] is None:
                    continue
                ins = o['fn'](eng)
                key = (o['pid'], o['idx'])
                if key in ranks:
                    s, _ = semval(*key)
                    ins.then_inc(s, 16 if o['dma'] else 1)

        block = stack.enter_context(nc.Block())

        @block.tensor
        def _(e):
            run('pe', e)

        @block.scalar
        def _(e):
            run('act', e)

        @block.vector
        def _(e):
            run('dve', e)

        @block.gpsimd
        def _(e):
            run('pool', e)

        @block.sync
        def _(e):
            run('sp', e)


class Arena:
    def __init__(self, nc, base, limit):
        self.nc, self.off, self.limit, self.n = nc, base, limit, 0

    def alloc(self, shape, dtype):
        size = int(np.prod(shape[1:])) * (2 if dtype == BF16 else 4)
        size = (size + 63) // 64 * 64
        self.n += 1
        t = self.nc.alloc_sbuf_tensor_at(f"t{self.n}_{self.off}", list(shape), dtype, offset=self.off)
        self.off += size
        assert self.off <= self.limit, (self.off, self.limit)
        return t


D = 1024
LAT = 2048
CTX = 256
NT = 18
NTOK = NT * 128
C = 32
NCH = 4
EPS = 1e-6
DFF = 2816
DEXP = 3584
NEXP = 8
BLKS = [(0, 256)] + [(256 + 512 * j, 512) for j in range(4)]
NCONST = 4 * 128 + 512
import os
DBG_NOROUTER = bool(os.environ.get('K_NOROUTER'))
DBG_SUB = os.environ.get('K_SUB', '')


def make_consts():
    j = np.arange(128)[:, None]
    i = np.arange(128)[None, :]
    same = (j // C) == (i // C)
    ident = np.eye(128, dtype=np.float32)
    triF = (same & (j <= i)).astype(np.float32)
    triB = (same & (j >= i)).astype(np.float32)
    ind = np.zeros((128, 128), np.float32)
    ind[np.arange(128), np.arange(128) // C] = 1.0
    mres = np.ones((128, 512), np.float32)
    mres[:, ::C] = 0.0
    return np.concatenate([ident, triF, triB, ind, mres], axis=1)


def build(n_layers=2, stop=None, nseq=2):
    nc = bass.Bass("TRN2", target_bir_lowering=False)

    def din(name, shape):
        return nc.dram_tensor(name, list(shape), F32, kind="ExternalInput").ap()
    x2 = din("x2", [2, LAT, D])
    ctx2 = din("ctx2", [2, CTX, D])
    c3T = din("c3T", [128, 24])
    cst = din("cst", [128, NCONST])
    w_mod = din("w_mod", [2, D, 6 * D])
    b_mod = din("b_mod", [2, 6 * D])
    norm1_g = din("norm1_g", [2, D])
    norm2_g = din("norm2_g", [2, D])
    w_in = din("w_in", [2, D, 4128])
    gate_w2 = din("gla_gate_w2", [2, 2, 16, 256])
    gate_b = din("gla_gate_b", [2, 2, 256])
    gla_ng = din("gla_norm_g", [2, 128])
    hg_ng = din("hgrn_norm_g", [2, 128])
    hgrn_lb = din("hgrn_lb", [2, 2, 512])
    w_out = din("w_out", [2, D, D])
    ffn_w1 = din("ffn_w1", [1, D, DFF])
    ffn_w3 = din("ffn_w3", [1, D, DFF])
    ffn_w2 = din("ffn_w2", [1, DFF, D])
    router = din("moe_router", [1, D, NEXP])
    moe_w1 = din("moe_w1", [1, NEXP, D, DEXP])
    moe_w3 = din("moe_w3", [1, NEXP, D, DEXP])
    moe_w2 = din("moe_w2", [1, NEXP, DEXP, D])
    fin_g = din("final_norm_g", [D])
    out2 = nc.dram_tensor("out2", [2, LAT, D], F32, kind="ExternalOutput").ap()
    dbg = None
    if stop is not None:
        dbg = nc.dram_tensor("dbg", [2, NTOK, D], F32, kind="ExternalOutput").ap()
    modsc = nc.dram_tensor("modsc", [2, 3, 6 * D], F32, kind="Internal").ap()
    xs = nc.dram_tensor("xs", [LAT, D], F32, kind="Internal").ap()

    S = Sched()
    with ExitStack() as st:
        base = (nc.SBUF_PARTITION_SIZE_BYTES - nc.sbuf_bytes_remaining + 63) // 64 * 64
        LIMIT = nc.SBUF_PARTITION_SIZE_BYTES
        A0 = Arena(nc, base, LIMIT)
        ps = [st.enter_context(nc.psum_tensor(f"ps{i}", [128, 512], F32)) for i in range(8)]
        psb = [p[:].bitcast(BF16) for p in ps]

        cf = A0.alloc([128, NCONST], F32)
        cb = A0.alloc([128, NCONST], BF16)
        identf = cf[:, 0:128]
        identb = cb[:, 0:128]
        maskF = cb[:, 128:256]
        maskB = cb[:, 256:384]
        indf = cf[:, 384:512]
        indb = cb[:, 384:512]
        mresb = cf[:, 512:1024]
        lball = A0.alloc([128, 16], F32)
        omlall = A0.alloc([128, 16], F32)
        ngcol = A0.alloc([128, 4], F32)
        small = A0.alloc([128, 64], F32)
        junk = A0.alloc([128, 1024], BF16)
        gates = A0.alloc([128, NT, NEXP], F32)
        hT = A0.alloc([128, 8, NTOK], BF16)
        base_regions = A0.off

        def eng_op(eng, name, R, W, **kw):
            S.op(eng, lambda e: getattr(e, name)(**kw), R, W)

        def ACT(out, in_, func, R, W, **kw):
            S.op('act', lambda e: e.activation(out=out, in_=in_, func=func, **kw), R, W)

        def MM(out, lhsT, rhs, R, W, start=True, stop=True, tp=None):
            if tp is None:
                S.op('pe', lambda e: e.matmul(out, lhsT=lhsT, rhs=rhs, start=start, stop=stop), R, W)
            else:
                S.op('pe', lambda e: e.matmul(out, lhsT=lhsT, rhs=rhs, start=start, stop=stop, tile_position=tp), R, W)

        def TR(out, in_, ident, R, W):
            S.op('pe', lambda e: e.transpose(out=out, in_=in_, identity=ident), R, W)

        def TT(eng, out, in0, in1, op, R, W):
            S.op(eng, lambda e: e.tensor_tensor(out=out, in0=in0, in1=in1, op=op), R, W)

        def TS(eng, out, in0, s1, s2, op0, op1, R, W):
            if s2 is None:
                S.op(eng, lambda e: e.tensor_scalar(out=out, in0=in0, scalar1=s1, scalar2=None, op0=op0), R, W)
            else:
                S.op(eng, lambda e: e.tensor_scalar(out=out, in0=in0, scalar1=s1, scalar2=s2, op0=op0, op1=op1), R, W)

        def STT(out, in0, scalar, in1, op0, op1, R, W):
            S.op('dve', lambda e: e.scalar_tensor_tensor(out=out, in0=in0, scalar=scalar, in1=in1, op0=op0, op1=op1), R, W)

        def DMA(q, out, in_, R, W, nc_ok=False):
            if nc_ok:
                S.dma(q, lambda e: e.dma_start(out=out, in_=in_, allow_slow_non_contiguous=True), R, W)
            else:
                S.dma(q, lambda e: e.dma_start(out=out, in_=in_), R, W)

        def rstd(ss, key):
            ACT(ss, ss, AF.Ln, [key], [key], bias=EPS)
            ACT(ss, ss, AF.Exp, [key], [key], scale=-0.5)

        DMA('sp', cf[:], cst, [], ['cf'])
        S.op('dve', lambda e: e.tensor_copy(out=cb[:], in_=cf[:]), ['cf'], ['cb'])
        CONST = ['cf', 'cb']
        DMA('sp', ngcol[:, 0:2], gla_ng.rearrange("l p -> p l"), [], ['ngcol'], nc_ok=True)
        DMA('sp', ngcol[:, 2:4], hg_ng.rearrange("l p -> p l"), [], ['ngcol'], nc_ok=True)
        DMA('sp', lball[:], hgrn_lb.rearrange("l d (h p) -> p (l d h)", p=128), [], ['lball'], nc_ok=True)
        e01 = small[:, 0:16]
        ACT(e01, lball[:], AF.Exp, ['lball'], ['e01'])
        ssum = small[:, 16:24]
        TT('dve', ssum, small[:, 0:8], small[:, 8:16], ALU.add, ['e01'], ['ssum'])
        S.op('dve', lambda e: e.reciprocal(out=ssum, in_=ssum), ['ssum'], ['ssum'])
        p0 = small[:, 24:32]
        p1 = small[:, 32:40]
        TT('dve', p0, small[:, 0:8], ssum, ALU.mult, ['e01', 'ssum'], ['p0'])
        TT('dve', p1, small[:, 8:16], ssum, ALU.mult, ['e01', 'ssum'], ['p1'])
        cum1 = small[:, 40:48]
        TT('dve', cum1, p0, p1, ALU.add, ['p0', 'p1'], ['cum1'])
        TT('dve', lball[:, 0:8], p0, p0, ALU.subtract, ['p0'], ['lball'])
        TT('dve', lball[:, 8:16], cum1, p0, ALU.subtract, ['cum1', 'p0'], ['lball'])
        TS('dve', lball[:], lball[:], 0.0, None, ALU.max, None, ['lball'], ['lball'])
        TS('dve', omlall[:], lball[:], -1.0, 1.0, ALU.mult, ALU.add, ['lball'], ['omlall'])

        Ap = Arena(nc, base_regions, LIMIT)
        ct = Ap.alloc([128, 24], F32)
        cs = Ap.alloc([128, 24], BF16)
        modv = Ap.alloc([3, 6 * D], F32)
        bm3 = Ap.alloc([3, 6 * D], F32)
        g13 = Ap.alloc([3, D], F32)
        g23 = Ap.alloc([3, D], F32)
        wm = [Ap.alloc([128, 8, 512], BF16) for _ in range(2)]
        DMA('sp', ct[:], c3T, [], ['ct'])
        ACT(cs[:], ct[:], AF.Silu, ['ct'], ['cs'])
        for l in range(n_layers):
            DMA('sp', bm3[:], b_mod[l, :].partition_broadcast(3), [], ['bm3'])
            DMA('sp', g13[:], norm1_g[l, :].partition_broadcast(3), [], ['g13'])
            DMA('sp', g23[:], norm2_g[l, :].partition_broadcast(3), [], ['g23'])
            for n in range(12):
                sl = n % 2
                DMA('pool', wm[sl][:], w_mod[l].rearrange("(k p) n -> p k n", p=128)[:, :, n * 512:(n + 1) * 512],
                    [], [f'wm{sl}'])
                for k in range(8):
                    MM(ps[sl][0:3, :], cs[:, 3 * k:3 * k + 3], wm[sl][:, k, :], ['cs', f'wm{sl}'], [f'ps{sl}'],
                       start=(k == 0), stop=(k == 7))
                TT('dve', modv[:, n * 512:(n + 1) * 512], ps[sl][0:3, :], bm3[:, n * 512:(n + 1) * 512], ALU.add,
                   [f'ps{sl}', 'bm3'], ['modv'])
            STT(modv[:, D:2 * D], modv[:, D:2 * D], 1.0, g13[:], ALU.add, ALU.mult, ['modv', 'g13'], ['modv'])
            STT(modv[:, 4 * D:5 * D], modv[:, 4 * D:5 * D], 1.0, g23[:], ALU.add, ALU.mult, ['modv', 'g23'], ['modv'])
            DMA('sp', modsc[l], modv[:], ['modv'], [f'modsc{l}'])
        S.barrier(lambda e: e.memset(small[0:1, 60:61], 0.0))

        def modvec(l, row, j):
            return modsc[l, row, j * D:(j + 1) * D].partition_broadcast(128)

        RA = base_regions
        acc = nc.alloc_sbuf_tensor_at("acc", [128, NT, D], F32, offset=RA)
        RB = RA + NT * D * 4
        RB_SIZE = 57344
        RC = RB + RB_SIZE

        def x_src(s, l, t):
            if l == 0:
                return ctx2[s, t * 128:(t + 1) * 128, :] if t < 2 else x2[s, (t - 2) * 128:(t - 1) * 128, :]
            return None

        def tile_cols(t):
            return slice(t * 128, (t + 1) * 128)

        for s in range(nseq):
            for l in range(n_layers):
                last = (l == n_layers - 1) and (l == 1)
                Ac = Arena(nc, RC, LIMIT)
                bcG = Ac.alloc([128, D], F32)
                bcS = Ac.alloc([128, D], F32)
                xt = [Ac.alloc([128, D], F32) for _ in range(2)]
                hf = Ac.alloc([128, D], F32)
                ssq = Ac.alloc([128, 2], F32)
                for t in range(NT):
                    if t == 0 or t == 2:
                        row = 2 if t == 0 else s
                        DMA('sp', bcS[:], modvec(l, row, 0), [f'modsc{l}'], ['bcS'])
                        DMA('sp', bcG[:], modvec(l, row, 1), [f'modsc{l}'], ['bcG'])
                    if l == 0:
                        xa = xt[t % 2]
                        xk = f'xt{t % 2}'
                        DMA('sp', xa[:], x_src(s, l, t), [], [xk])
                        xin = xa[:]
                    else:
                        xin = acc[:, t, :]
                        xk = f'acc{t}'
                        if t >= 2:
                            DMA('sp', xs[(t - 2) * 128:(t - 1) * 128, :], acc[:, t, :], [xk], [f'xs{t}'])
                    ACT(junk[:], xin, AF.Square, [xk], ['ssq'], scale=D ** -0.5, accum_out=ssq[:, 0:1])
                    rstd(ssq[:, 0:1], 'ssq')
                    STT(hf[:], xin, ssq[:, 0:1], bcG[:], ALU.mult, ALU.mult, [xk, 'ssq', 'bcG'], ['hf'])
                    TT('pool', hf[:], hf[:], bcS[:], ALU.add, ['hf', 'bcS'], ['hf'])
                    for k in range(8):
                        TR(ps[(k // 4)][:, (k % 4) * 128:(k % 4 + 1) * 128], hf[:, k * 128:(k + 1) * 128], identf,
                           ['hf'] + CONST, [f'ps{k // 4}'])
                    for hh in range(2):
                        ACT(hT[:, 4 * hh:4 * hh + 4, tile_cols(t)], ps[hh][:].rearrange("p (k c) -> p k c", c=128), AF.Copy,
                            [f'ps{hh}'], [f'hT{t}'])
                S.barrier(lambda e: e.memset(small[0:1, 60:61], 0.0))

                mixT = nc.alloc_sbuf_tensor_at(f"mixT_{s}_{l}", [128, 8, NTOK], BF16, offset=RB)
                wout = nc.alloc_sbuf_tensor_at(f"wout_{s}_{l}", [128, 8, D], BF16, offset=RB + 8 * NTOK * 2)
                Aa = Arena(nc, RA, RB)
                Ac = Arena(nc, RC, LIMIT)
                sgT = Aa.alloc([128, NTOK], BF16)
                qtT = [Aa.alloc([128, NTOK], BF16) for _ in range(2)]
                ktT = [Aa.alloc([128, NTOK], BF16) for _ in range(2)]
                kdec = [Aa.alloc([128, NT, 128], BF16) for _ in range(2)]
                vbf = Aa.alloc([128, NT, 128], BF16)
                o_fb = [Aa.alloc([128, NT, 128], F32) for _ in range(2)]
                a_all = [Aa.alloc([128, NT * NCH], F32) for _ in range(2)]
                win = [Aa.alloc([128, 8, 640], BF16)]
                w2a = [Aa.alloc([32, 256], BF16) for _ in range(2)]
                Sbf = [Aa.alloc([128, NCH, 128], BF16) for _ in range(2)]
                vblk = [Aa.alloc([128, NCH, 128], BF16) for _ in range(2)]
                PT = [Aa.alloc([128, 128], BF16) for _ in range(2)]
                tmpP = nc.alloc_sbuf_tensor_at(f"tmpP_{s}_{l}", [128, LAT], BF16, offset=Ac.off)
                qf = Ac.alloc([128, 512], F32)
                kf = [Ac.alloc([128, 512], F32) for _ in range(2)]
                uu = [Ac.alloc([128, 512], F32) for _ in range(2)]
                bF = Ac.alloc([128, 512], F32)
                bb = Ac.alloc([128, 512], F32)
                ee = Ac.alloc([128, 512], F32)
                rr = ee
                e2 = Ac.alloc([128, 512], F32)
                t1 = Ac.alloc([128, 512], F32)
                vTb = Ac.alloc([128, 512], BF16)
                kdT = Ac.alloc([128, 512], BF16)
                otot = [Ac.alloc([128, 128], F32) for _ in range(2)]
                onb = [Ac.alloc([128, 128], BF16) for _ in range(2)]
                wgr = Aa.alloc([128, 8, 32], BF16)
                Sst = [[Aa.alloc([128, 128], F32) for _ in range(2)], [Ac.alloc([128, 128], F32) for _ in range(2)]]
                oss = Ac.alloc([128, 2], F32)
                gfa = [Ac.alloc([32, NTOK], BF16) for _ in range(2)]

                DMA('pool', wout[:], w_out[l].rearrange("(k p) n -> p k n", p=128), [], ['wout'])
                winl = w_in[l].rearrange("(k p) n -> p k n", p=128)
                DMA('pool', wgr[:], winl[:, :, 1536:1568], [], ['wgr'])
                for d in range(2):
                    eng_op('pool', 'memset', [], [f'gfa{d}'], ap=gfa[d][:], constant=1.0)
                    DMA('pool', w2a[d][0:16, :], gate_w2[l, d], [], [f'w2a{d}'])
                    DMA('pool', w2a[d][16:17, :], gate_b[l, d:d + 1, :], [], [f'w2a{d}'])
                for bi, (b0, bn) in enumerate(BLKS):
                    for d in range(2):
                        for k in range(8):
                            MM(ps[d][0:16, 0:bn], wgr[:, k, 16 * d:16 * d + 16], hT[:, k, b0:b0 + bn],
                               ['wgr'] + [f'hT{t}' for t in range(b0 // 128, (b0 + bn) // 128)], [f'ps{d}'],
                               start=(k == 0), stop=(k == 7))
                        ACT(gfa[d][0:16, b0:b0 + bn], ps[d][0:16, 0:bn], AF.Copy, [f'ps{d}'], [f'gfa{d}'])

                def hblk(k, cm, bi):
                    b0, bn = BLKS[bi]
                    return hT[:, k, b0:b0 + bn]

                ALLH = [f'hT{t}' for t in range(NT)]
                for hd in range(8):
                    gla = hd < 4
                    if hd == 4:
                        LATK = [f'hT{t}' for t in range(2, NT)]
                        for k in range(8):
                            ACT(tmpP[:].rearrange("p (w r) -> p w r", w=64), hT[:, k, 256:NTOK].rearrange("p (r w) -> p w r", w=64), AF.Copy,
                                LATK, ['qf', 'kf0'])
                            eng_op('pool', 'tensor_copy', ['qf', 'kf0'], LATK, out=hT[:, k, 256:NTOK], in_=tmpP[:])
                    h = hd % 4
                    dk = 64 if gla else 128
                    gs = -1.0 / 16 if gla else -1.0
                    wslot = 0
                    wt = win[wslot]
                    wk = f'win{wslot}'
                    if gla:
                        pieces = [(64 * h, 64, 0), (256 + 64 * h, 64, 64), (512 + 128 * h, 128, 128), (1024 + 128 * h, 128, 256)]
                    else:
                        pieces = [(1568 + 128 * h, 128, 0), (2080 + 128 * h, 128, 128), (2592 + 128 * h, 128, 256),
                                  (3104 + 128 * h, 128, 384), (3616 + 128 * h, 128, 512)]
                    for (c0, w, o) in pieces:
                        DMA('pool', wt[:, :, o:o + w], winl[:, :, c0:c0 + w], [], [wk])
                    ngc = ngcol[:, l:l + 1] if gla else ngcol[:, 2 + l:3 + l]

                    for bi, (b0, bn) in enumerate(BLKS):
                        t0 = b0 // 128
                        ntl = bn // 128

                        def emit_proj(bj):
                            pb0, pbn = BLKS[bj]

                            def proj(bank, M, co):
                                for k in range(8):
                                    MM(ps[bank][0:M, 0:pbn], wt[:, k, co:co + M], hblk(k, not gla, bj), [wk] + ALLH, [f'ps{bank}'],
                                       start=(k == 0), stop=(k == 7))
                            if gla:
                                proj(0, 64, 0)
                                proj(1, 64, 64)
                                proj(2, 128, 128)
                                proj(3, 128, 256)
                                for d in range(2):
                                    MM(ps[4 + d][0:64, 0:pbn], w2a[d][0:17, 64 * h:64 * h + 64], gfa[d][0:17, pb0:pb0 + pbn],
                                       [f'w2a{d}', f'gfa{d}'], [f'ps{4 + d}'])
                            else:
                                proj(0, 128, 0)
                                proj(1, 128, 128)
                                proj(4, 128, 256)
                                proj(2, 128, 384)
                                proj(3, 128, 512)
                        if bi == 0:
                            emit_proj(0)
                        if gla:
                            TS('dve', qf[0:64, 0:bn], ps[0][0:64, 0:bn], 0.125, None, ALU.mult, None, ['ps0'], ['qf'])
                            eng_op('dve', 'tensor_copy', ['ps1'], ['kf0'], out=kf[0][0:64, 0:bn], in_=ps[1][0:64, 0:bn])
                            kfd = [kf[0], kf[0]]
                            kfk = ['kf0', 'kf0']
                            for d in range(2):
                                ACT(ee[0:64, 0:bn], ps[4 + d][0:64, 0:bn], AF.Exp, [f'ps{4 + d}'], ['ee'], scale=-1.0)
                                ACT(uu[d][0:64, 0:bn], ee[0:64, 0:bn], AF.Ln, ['ee'], [f'uu{d}'], bias=1.0)
                        else:
                            ACT(qf[:, 0:bn], ps[0][:, 0:bn], AF.Silu, ['ps0'], ['qf'])
                            kfd = kf
                            kfk = ['kf0', 'kf1']
                            for d in range(2):
                                bank = 1 if d == 0 else 4
                                col = l * 8 + d * 4 + h
                                ACT(ee[:, 0:bn], ps[bank][:, 0:bn], AF.Exp, [f'ps{bank}'], ['ee'], scale=-1.0)
                                ACT(bb[:, 0:bn], ee[:, 0:bn], AF.Ln, ['ee'], ['bb'], bias=1.0)
                                ACT(t1[:, 0:bn], ee[:, 0:bn], AF.Ln, ['ee', 'lball'], ['t1'], bias=1.0, scale=lball[:, col:col + 1])
                                TT('pool', uu[d][:, 0:bn], bb[:, 0:bn], t1[:, 0:bn], ALU.subtract, ['bb', 't1'], [f'uu{d}'])
                                ACT(e2[:, 0:bn], bb[:, 0:bn], AF.Exp, ['bb'], ['e2'], scale=-1.0)
                                STT(kf[d][:, 0:bn], ee[:, 0:bn], omlall[:, col:col + 1], e2[:, 0:bn], ALU.mult, ALU.mult,
                                    ['ee', 'e2', 'omlall'], [f'kf{d}'])
                        ACT(vTb[:, 0:bn], ps[2][:, 0:bn], AF.Copy, ['ps2'], ['vTb'])
                        ACT(sgT[:, b0:b0 + bn], ps[3][:, 0:bn], AF.Silu, ['ps3'], [f'sgT{bi}'])
                        if bi + 1 < len(BLKS):
                            emit_proj(bi + 1)
                        for j in range(ntl):
                            TR(psb[6][:, j * 128:(j + 1) * 128], vTb[:, j * 128:(j + 1) * 128], identb, ['vTb'] + CONST, ['ps6'])
                        eng_op('dve', 'tensor_copy', ['ps6'], [f'vbf{t}' for t in range(t0, t0 + ntl)],
                               out=vbf[:, t0:t0 + ntl, :], in_=psb[6][:, 0:bn].rearrange("p (j c) -> p j c", c=128))
                        for d in range(2):
                            nchb = bn // C
                            eng_op('dve', 'tensor_tensor_scan', [f'uu{d}'] + CONST, ['bF'], out=bF[0:dk, 0:bn], data0=mresb[0:dk, 0:bn],
                                   data1=uu[d][0:dk, 0:bn], initial=0.0, op0=ALU.mult, op1=ALU.add)
                            blast = bF[0:dk, 0:bn].rearrange("p (c j) -> p c j", j=C)[:, :, C - 1:C]
                            bl_bc = blast.to_broadcast([dk, nchb, C])
                            bF3 = bF[0:dk, 0:bn].rearrange("p (c j) -> p c j", j=C)
                            rr3 = rr[0:dk, 0:bn].rearrange("p (c j) -> p c j", j=C)
                            if d == 0:
                                bsrc = bF
                                bkey = 'bF'
                                TT('dve', rr3, bl_bc, bF3, ALU.subtract, ['bF'], ['ee'])
                            else:
                                TT('dve', rr[0:dk, 0:bn], bF[0:dk, 0:bn], uu[d][0:dk, 0:bn], ALU.subtract, ['bF', f'uu{d}'], ['ee'])
                                bb3 = bb[0:dk, 0:bn].rearrange("p (c j) -> p c j", j=C)
                                TT('pool', bb3, bl_bc, rr3, ALU.subtract, ['bF', 'ee'], ['bb'])
                                bsrc = bb
                                bkey = 'bb'
                            ACT(a_all[d][0:dk, (b0 // C):(b0 // C) + nchb], bF[0:dk, 0:bn].rearrange("p (c j) -> p c j", j=C)[:, :, C - 1],
                                AF.Exp, ['bF'], [f'a{d}'], scale=gs)
                            ACT(e2[0:dk, 0:bn], bsrc[0:dk, 0:bn], AF.Exp, [bkey], ['e2'], scale=gs)
                            TT('dve', qtT[d][0:dk, b0:b0 + bn], qf[0:dk, 0:bn], e2[0:dk, 0:bn], ALU.mult, ['qf', 'e2'], [f'qtT{d}_{bi}'])
                            ACT(t1[0:dk, 0:bn], bsrc[0:dk, 0:bn], AF.Exp, [bkey], ['t1'], scale=-gs)
                            TT('dve', ktT[d][0:dk, b0:b0 + bn], kfd[d][0:dk, 0:bn], t1[0:dk, 0:bn], ALU.mult, [kfk[d], 't1'], [f'ktT{d}_{bi}'])
                            ACT(e2[0:dk, 0:bn], rr[0:dk, 0:bn], AF.Exp, ['ee'], ['e2'], scale=gs)
                            TT('pool', kdT[0:dk, 0:bn], kfd[d][0:dk, 0:bn], e2[0:dk, 0:bn], ALU.mult, [kfk[d], 'e2'], ['kdT'])
                            for j in range(ntl):
                                TR(psb[7][:, j * 128:j * 128 + dk], kdT[0:dk, j * 128:(j + 1) * 128], identb[0:dk, 0:dk], ['kdT'] + CONST, ['ps7'])
                            ACT(kdec[d][:, t0:t0 + ntl, 0:dk], psb[7][:, 0:bn].rearrange("p (j c) -> p j c", c=128)[:, :, 0:dk], AF.Copy,
                                ['ps7'], [f'kdec{d}_{t}' for t in range(t0, t0 + ntl)])

                    orders = [list(range(NT)), [1, 0] + list(range(NT - 1, 1, -1))]
                    corders = [list(range(NCH)), list(range(NCH - 1, -1, -1))]
                    masks = [maskF, maskB]
                    PKV = [5, 6]
                    PSC = [0, 3]
                    PO = [1, 4]
                    for d in range(2):
                        eng_op('pool', 'memset', [], [f'Sst{d}_0'], ap=Sst[d][0][:], constant=0.0)
                        eng_op('pool', 'memset', [], [f'Sbf{d}_0'], ap=Sbf[d][:, 0, :], constant=0.0)
                    sp = [0, 0]
                    for it in range(NT):
                        tt = [orders[d][it] for d in range(2)]
                        need = [not (last and tt[d] < 2) for d in range(2)]
                        bis = [0 if tt[d] < 2 else 1 + (tt[d] - 2) // 4 for d in range(2)]
                        for d in range(2):
                            t = tt[d]
                            TT('dve', vblk[d][:], vbf[:, t, :].unsqueeze(1).to_broadcast([128, NCH, 128]),
                               indf[:, 0:NCH].unsqueeze(2).to_broadcast([128, NCH, 128]), ALU.mult, [f'vbf{t}'] + CONST, [f'vblk{d}'])
                            MM(ps[PKV[d]][0:dk, :], kdec[d][:, t, 0:dk], vblk[d][:].rearrange("p c e -> p (c e)"), [f'kdec{d}_{t}', f'vblk{d}'], [f'ps{PKV[d]}'])
                        for d in range(2):
                            t = tt[d]
                            if need[d]:
                                MM(ps[PSC[d]][:, 0:128], ktT[d][0:dk, tile_cols(t)], qtT[d][0:dk, tile_cols(t)],
                                   [f'ktT{d}_{bis[d]}', f'qtT{d}_{bis[d]}'], [f'ps{PSC[d]}'])
                        for d in range(2):
                            t = tt[d]
                            if need[d]:
                                TT('dve', PT[d][:], ps[PSC[d]][:, 0:128], masks[d], ALU.mult, [f'ps{PSC[d]}'] + CONST, [f'PT{d}'])
                                MM(ps[PO[d]][:, 0:128], PT[d][:], vbf[:, t, :], [f'PT{d}', f'vbf{t}'], [f'ps{PO[d]}'], start=True, stop=False)
                        for ci in range(NCH):
                            for d in range(2):
                                t = tt[d]
                                c = corders[d][ci]
                                if need[d]:
                                    MM(ps[PO[d]][C * c:C * c + C, 0:128], qtT[d][0:dk, t * 128 + C * c:t * 128 + C * c + C], Sbf[d][0:dk, ci, :],
                                       [f'qtT{d}_{bis[d]}', f'Sbf{d}_{ci}'], [f'ps{PO[d]}'], start=False, stop=True, tp=(0, C * c))
                            for d in range(2):
                                t = tt[d]
                                c = corders[d][ci]
                                p = sp[d]
                                STT(Sst[d][1 - p][0:dk, :], Sst[d][p][0:dk, :], a_all[d][0:dk, t * NCH + c:t * NCH + c + 1],
                                    ps[PKV[d]][0:dk, c * 128:(c + 1) * 128], ALU.mult, ALU.add, [f'Sst{d}_{p}', f'a{d}', f'ps{PKV[d]}'], [f'Sst{d}_{1 - p}'])
                                sp[d] = 1 - p
                            for d in range(2):
                                nslot = (ci + 1) if ci < NCH - 1 else 0
                                ACT(Sbf[d][0:dk, nslot, :], Sst[d][sp[d]][0:dk, :], AF.Copy, [f'Sst{d}_{sp[d]}'], [f'Sbf{d}_{nslot}'])
                        for d in range(2):
                            if need[d]:
                                ACT(o_fb[d][:, tt[d], :], ps[PO[d]][:, 0:128], AF.Copy, [f'ps{PO[d]}'], [f'o{d}_{tt[d]}'])
                    for t in range(NT):
                        if last and t < 2:
                            continue
                        bi = 0 if t < 2 else 1 + (t - 2) // 4
                        es = t % 2
                        epb = 2 if es == 0 else 7
                        TT('pool', otot[es][:], o_fb[0][:, t, :], o_fb[1][:, t, :], ALU.add, [f'o0_{t}', f'o1_{t}'], [f'otot{es}'])
                        ACT(junk[:, 0:128], otot[es][:], AF.Square, [f'otot{es}'], [f'oss{es}'], scale=128 ** -0.5, accum_out=oss[:, es:es + 1])
                        rstd(oss[:, es:es + 1], f'oss{es}')
                        TS('dve', onb[es][:], otot[es][:], oss[:, es:es + 1], None, ALU.mult, None, [f'otot{es}', f'oss{es}'], [f'onb{es}'])
                        TR(psb[epb][:, 0:128], onb[es][:], identb, [f'onb{es}'] + CONST, [f'ps{epb}'])
                        if gla or t < 2:
                            STT(mixT[:, hd, tile_cols(t)], psb[epb][:, 0:128], ngc, sgT[:, tile_cols(t)], ALU.mult, ALU.mult,
                                [f'ps{epb}', f'sgT{bi}', 'ngcol'], [f'mixT{hd}'])
                        else:
                            j = t - 2
                            ov = mixT[:, hd, 256:NTOK].rearrange("p (r w) -> p w r", w=64)[:, 4 * j:4 * j + 4, :]
                            STT(ov, psb[epb][:, 0:128].rearrange("p (w r) -> p w r", w=4), ngc,
                                sgT[:, tile_cols(t)].rearrange("p (w r) -> p w r", w=4), ALU.mult, ALU.mult,
                                [f'ps{epb}', f'sgT{bi}', 'ngcol'], [f'mixT{hd}'])
                S.barrier(lambda e: e.memset(small[0:1, 60:61], 0.0))

                Ac = Arena(nc, RC, LIMIT)
                bcG1 = Ac.alloc([128, D], F32)
                bcG2 = Ac.alloc([128, D], F32)
                bcS2 = Ac.alloc([128, D], F32)
                xt = [Ac.alloc([128, D], F32) for _ in range(2)]
                tmp = Ac.alloc([128, D], F32)
                hf = Ac.alloc([128, D], F32)
                hTf = Ac.alloc([128, 8, 128], F32)
                rtf = Ac.alloc([128, 8, NEXP], F32)
                lg = Ac.alloc([128, 4 * NEXP], F32)
                ssq = Ac.alloc([128, 8], F32)
                moe = (l % 2 == 1)
                if moe:
                    DMA('sp', rtf[:], router[0].rearrange("(k p) e -> p k e", p=128), [], ['rtf'], nc_ok=True)
                MIXK = [f'mixT{hd}' for hd in range(8)]
                for t in range(NT):
                    if last and t < 2:
                        continue
                    if t == 0 or t == 2:
                        row = 2 if t == 0 else s
                        DMA('sp', bcG1[:], modvec(l, row, 2), [f'modsc{l}'], ['bcG1'])
                        DMA('sp', bcS2[:], modvec(l, row, 3), [f'modsc{l}'], ['bcS2'])
                        DMA('sp', bcG2[:], modvec(l, row, 4), [f'modsc{l}'], ['bcG2'])
                    xa = xt[t % 2]
                    xk = f'xt{t % 2}'
                    if l == 0:
                        DMA('sp', xa[:], x_src(s, l, t), [], [xk])
                    else:
                        DMA('sp', xa[:], xs[(t - 2) * 128:(t - 1) * 128, :], [f'xs{t}'], [xk])
                    for nh in range(2):
                        for k in range(8):
                            MM(ps[nh][:, :], mixT[:, k, tile_cols(t)], wout[:, k, nh * 512:(nh + 1) * 512], MIXK + ['wout'], [f'ps{nh}'],
                               start=(k == 0), stop=(k == 7))
                        TT('dve', tmp[:, nh * 512:(nh + 1) * 512], ps[nh][:, :], bcG1[:, nh * 512:(nh + 1) * 512], ALU.mult,
                           [f'ps{nh}', 'bcG1'], ['tmp'])
                    TT('pool', acc[:, t, :], xa[:], tmp[:], ALU.add, [xk, 'tmp'], [f'acc{t}'])
                    ak = f'acc{t}'
                    ACT(junk[:], acc[:, t, :], AF.Square, [ak], ['ssq'], scale=D ** -0.5, accum_out=ssq[:, 0:1])
                    rstd(ssq[:, 0:1], 'ssq')
                    STT(hf[:], acc[:, t, :], ssq[:, 0:1], bcG2[:], ALU.mult, ALU.mult, [ak, 'ssq', 'bcG2'], ['hf'])
                    TT('pool', hf[:], hf[:], bcS2[:], ALU.add, ['hf', 'bcS2'], ['hf'])
                    for k in range(8):
                        TR(ps[2 + (k // 4)][:, (k % 4) * 128:(k % 4 + 1) * 128], hf[:, k * 128:(k + 1) * 128], identf,
                           ['hf'] + CONST, [f'ps{2 + k // 4}'])
                    for hh in range(2):
                        ACT(hT[:, 4 * hh:4 * hh + 4, tile_cols(t)], ps[2 + hh][:].rearrange("p (k c) -> p k c", c=128), AF.Copy,
                            [f'ps{2 + hh}'], [f'hT{t}'])
                    if moe and t >= 2 and not DBG_NOROUTER:
                        for hh in range(2):
                            ACT(hTf[:, 4 * hh:4 * hh + 4, :], ps[2 + hh][:].rearrange("p (k c) -> p k c", c=128), AF.Copy, [f'ps{2 + hh}'], ['hTf'])
                        if DBG_SUB == '1':
                            continue
                        for k in range(8):
                            MM(ps[4][:, 0:NEXP], hTf[:, k, :], rtf[:, k, :], ['hTf', 'rtf'], ['ps4'], start=(k == 0), stop=(k == 7))
                        if DBG_SUB == '2':
                            continue
                        L = lg[:, 0:8]
                        EQ1 = lg[:, 8:16]
                        L2 = lg[:, 16:24]
                        EQ2 = lg[:, 24:32]
                        eng_op('dve', 'tensor_copy', ['ps4'], ['lg'], out=L, in_=ps[4][:, 0:NEXP])
                        eng_op('dve', 'reduce_max', ['lg'], ['ssq'], out=ssq[:, 1:2], in_=L, axis=mybir.AxisListType.X)
                        TS('dve', EQ1, L, ssq[:, 1:2], None, ALU.is_equal, None, ['lg', 'ssq'], ['lg'])
                        STT(L2, EQ1, -1e30, L, ALU.mult, ALU.add, ['lg'], ['lg'])
                        eng_op('dve', 'reduce_max', ['lg'], ['ssq'], out=ssq[:, 2:3], in_=L2, axis=mybir.AxisListType.X)
                        TS('dve', EQ2, L2, ssq[:, 2:3], None, ALU.is_equal, None, ['lg', 'ssq'], ['lg'])
                        if DBG_SUB == '3':
                            continue
                        TT('dve', ssq[:, 3:4], ssq[:, 2:3], ssq[:, 1:2], ALU.subtract, ['ssq'], ['ssq'])
                        ACT(ssq[:, 4:5], ssq[:, 3:4], AF.Exp, ['ssq'], ['ssq'])
                        TS('dve', ssq[:, 5:6], ssq[:, 4:5], 1.0, None, ALU.add, None, ['ssq'], ['ssq'])
                        eng_op('dve', 'reciprocal', ['ssq'], ['ssq'], out=ssq[:, 5:6], in_=ssq[:, 5:6])
                        TT('dve', ssq[:, 6:7], ssq[:, 4:5], ssq[:, 5:6], ALU.mult, ['ssq'], ['ssq'])
                        TS('dve', gates[:, t, :], EQ1, ssq[:, 5:6], None, ALU.mult, None, ['lg', 'ssq'], [f'gates{t}'])
                        STT(gates[:, t, :], EQ2, ssq[:, 6:7], gates[:, t, :], ALU.mult, ALU.add, ['lg', 'ssq', f'gates{t}'], [f'gates{t}'])
                if stop == ('mix', l):
                    for t in range(NT):
                        DMA('sp', dbg[s, t * 128:(t + 1) * 128, :], acc[:, t, :], [f'acc{t}'], [f'dbg{s}_{t}'])
                    S.barrier(lambda e: e.memset(small[0:1, 60:61], 0.0))
                    break
                S.barrier(lambda e: e.memset(small[0:1, 60:61], 0.0))

                Ab = Arena(nc, RB, RC)
                GS = 4 if moe else 2
                w1g = [Ab.alloc([128, 8, GS * 128], BF16) for _ in range(2)]
                w3g = [Ab.alloc([128, 8, GS * 128], BF16) for _ in range(2)]
                w2g = [Ab.alloc([128, GS, D], BF16) for _ in range(2)]
                aT = [Ab.alloc([128, GS, 512], BF16) for _ in range(2)]
                Ac2 = Arena(nc, RC, LIMIT)
                s1 = [Ac2.alloc([128, 512], F32) for _ in range(2)]
                bcg = [Ac2.alloc([128, D], F32) for _ in range(2)]
                tm = [Ac2.alloc([128, D], F32) for _ in range(2)]
                DMA('sp', bcg[0][:], modvec(l, 2, 5), [f'modsc{l}'], ['bcg0'])
                DMA('sp', bcg[1][:], modvec(l, s, 5), [f'modsc{l}'], ['bcg1'])
                nE = NEXP if moe else 1
                FF = DEXP if moe else DFF
                ngroups = FF // (GS * 128)
                tblocks = ([] if last else [(0, 256)]) + [(256 + 512 * j, 512) for j in range(4)]
                groups = [(ex, g) for ex in range(nE) for g in range(ngroups)]

                def emit_wdma(gidx):
                    ex, g = groups[gidx]
                    sl = gidx % 2
                    W1 = moe_w1[0, ex] if moe else ffn_w1[0]
                    W3 = moe_w3[0, ex] if moe else ffn_w3[0]
                    W2 = moe_w2[0, ex] if moe else ffn_w2[0]
                    f0 = g * GS * 128
                    DMA('pool', w1g[sl][:], W1.rearrange("(k p) n -> p k n", p=128)[:, :, f0:f0 + GS * 128], [], [f'w1g{sl}'])
                    DMA('pool', w3g[sl][:], W3.rearrange("(k p) n -> p k n", p=128)[:, :, f0:f0 + GS * 128], [], [f'w3g{sl}'])
                    DMA('pool', w2g[sl][:], W2[f0:f0 + GS * 128, :].rearrange("(g p) n -> p g n", p=128), [], [f'w2g{sl}'])

                items = [(gidx, bidx) for gidx in range(len(groups)) for bidx in range(len(tblocks))]

                def stage1(ii):
                    gidx, bidx = items[ii]
                    sl = gidx % 2
                    asl = ii % 2
                    b0, bn = tblocks[bidx]
                    hk = [f'hT{t}' for t in range(b0 // 128, (b0 + bn) // 128)]
                    for f in range(GS):
                        pb = 2 * (f % 2)
                        for k in range(8):
                            MM(ps[pb][:, 0:bn], w1g[sl][:, k, f * 128:(f + 1) * 128], hT[:, k, b0:b0 + bn], [f'w1g{sl}'] + hk, [f'ps{pb}'],
                               start=(k == 0), stop=(k == 7))
                        for k in range(8):
                            MM(ps[pb + 1][:, 0:bn], w3g[sl][:, k, f * 128:(f + 1) * 128], hT[:, k, b0:b0 + bn], [f'w3g{sl}'] + hk, [f'ps{pb + 1}'],
                               start=(k == 0), stop=(k == 7))
                        ACT(s1[f % 2][:, 0:bn], ps[pb][:, 0:bn], AF.Silu, [f'ps{pb}'], [f's1{f % 2}'])
                        TT('dve', aT[asl][:, f, 0:bn], ps[pb + 1][:, 0:bn], s1[f % 2][:, 0:bn], ALU.mult,
                           [f'ps{pb + 1}', f's1{f % 2}'], [f'aT{asl}'])

                tcount = [0]

                def stage2(ii):
                    gidx, bidx = items[ii]
                    ex, g = groups[gidx]
                    sl = gidx % 2
                    asl = ii % 2
                    b0, bn = tblocks[bidx]
                    for j in range(bn // 128):
                        t = b0 // 128 + j
                        tsl = tcount[0] % 2
                        tcount[0] += 1
                        for nh in range(2):
                            pbk = 4 + 2 * tsl + nh
                            for f in range(GS):
                                MM(ps[pbk][:, :], aT[asl][:, f, j * 128:(j + 1) * 128], w2g[sl][:, f, nh * 512:(nh + 1) * 512],
                                   [f'aT{asl}', f'w2g{sl}'], [f'ps{pbk}'], start=(f == 0), stop=(f == GS - 1))
                            gsc = gates[:, t, ex:ex + 1] if moe else 1.0
                            bg = bcg[0 if t < 2 else 1]
                            STT(tm[tsl][:, nh * 512:(nh + 1) * 512], ps[pbk][:, :], gsc, bg[:, nh * 512:(nh + 1) * 512], ALU.mult, ALU.mult,
                                [f'ps{pbk}', f'gates{t}', 'bcg0', 'bcg1'], [f'tm{tsl}'])
                        TT('pool', acc[:, t, :], acc[:, t, :], tm[tsl][:], ALU.add, [f'acc{t}', f'tm{tsl}'], [f'acc{t}'])

                emit_wdma(0)
                if len(groups) > 1:
                    emit_wdma(1)
                stage1(0)
                for ii in range(len(items)):
                    if ii + 1 < len(items):
                        stage1(ii + 1)
                    stage2(ii)
                    gidx, bidx = items[ii]
                    if bidx == len(tblocks) - 1 and gidx + 2 < len(groups):
                        emit_wdma(gidx + 2)
                if stop == ('ffn', l):
                    for t in range(NT):
                        DMA('sp', dbg[s, t * 128:(t + 1) * 128, :], acc[:, t, :], [f'acc{t}'], [f'dbg{s}_{t}'])
                    S.barrier(lambda e: e.memset(small[0:1, 60:61], 0.0))
                    break
                S.barrier(lambda e: e.memset(small[0:1, 60:61], 0.0))
            else:
                Ac = Arena(nc, RC, LIMIT)
                fg = Ac.alloc([128, D], F32)
                ot = [Ac.alloc([128, D], F32) for _ in range(2)]
                ssq = Ac.alloc([128, 2], F32)
                DMA('sp', fg[:], fin_g.partition_broadcast(128), [], ['fg'])
                for t in range(2, NT):
                    ACT(junk[:], acc[:, t, :], AF.Square, [f'acc{t}'], ['ssq'], scale=D ** -0.5, accum_out=ssq[:, 0:1])
                    rstd(ssq[:, 0:1], 'ssq')
                    STT(ot[t % 2][:], acc[:, t, :], ssq[:, 0:1], fg[:], ALU.mult, ALU.mult, [f'acc{t}', 'ssq', 'fg'], [f'ot{t % 2}'])
                    DMA('sp', out2[s, (t - 2) * 128:(t - 1) * 128, :], ot[t % 2][:], [f'ot{t % 2}'], [f'out{s}_{t}'])
                S.barrier(lambda e: e.memset(small[0:1, 60:61], 0.0))
        S.op('sp', None, [k for k in S.last_w if isinstance(k, str) and (k.startswith('out') or k.startswith('dbg'))], [])
        S.emit(nc, st)
    return nc


_CACHE = {}


def make_in_maps(inputs, ncores=8):
    f = lambda a: np.ascontiguousarray(np.asarray(a, dtype=np.float32))
    cst = make_consts()
    shared = {k: f(inputs[k]) for k in ("w_mod", "b_mod", "norm1_g", "norm2_g", "w_in", "gla_gate_w2", "gla_gate_b",
                                        "gla_norm_g", "hgrn_norm_g", "hgrn_lb", "w_out", "ffn_w1", "ffn_w3", "ffn_w2",
                                        "moe_router", "moe_w1", "moe_w3", "moe_w2", "final_norm_g")}
    x, c, ctx, c_ctx = f(inputs["x"]), f(inputs["c"]), f(inputs["ctx"]), f(inputs["c_ctx"])
    maps = []
    for i in range(ncores):
        c3 = np.stack([c[2 * i], c[2 * i + 1], c_ctx], axis=0)
        c3T = np.ascontiguousarray(c3.reshape(3, 8, 128).transpose(2, 1, 0).reshape(128, 24))
        m = dict(shared)
        m.update(x2=np.ascontiguousarray(x[2 * i:2 * i + 2]), ctx2=np.ascontiguousarray(ctx[2 * i:2 * i + 2]), c3T=c3T, cst=cst)
        maps.append(m)
    return maps


def kernel(**inputs):
    if 'nc' not in _CACHE:
        _CACHE['nc'] = build()
    nc = _CACHE['nc']
    maps = make_in_maps(inputs)
    res = run_bass_kernel_spmd(nc, maps, core_ids=list(range(8)))
    return np.concatenate([r["out2"] for r in res.results], axis=0).astype(np.float32)
```

```python
from contextlib import ExitStack
import numpy as np
import concourse.bass as bass
import concourse.mybir as mybir
from concourse.bass_utils import run_bass_kernel_spmd

F32 = mybir.dt.float32
BF16 = mybir.dt.bfloat16
AF = mybir.ActivationFunctionType
ALU = mybir.AluOpType

CE = ('pe', 'act', 'dve', 'pool', 'sp')
EPOCH = 12000
NLANE = 8


class Sched:
    def __init__(self):
        self.ops = {e: [] for e in CE}
        self.prod = {}
        self.last_w = {}
        self.readers = {}
        self.seen = {e: {} for e in CE}
        self.lane_rr = {'sp': 0, 'pool': 0}
        self.marked = set()
        self.barrier_dep = None

    def _deps(self, R, W):
        need = {}

        def add(p, i):
            if need.get(p, 0) < i:
                need[p] = i
        for k in R:
            lw = self.last_w.get(k)
            if lw:
                add(*lw)
        for k in W:
            lw = self.last_w.get(k)
            if lw:
                add(*lw)
            for p, i in self.readers.get(k, {}).items():
                add(p, i)
        if self.barrier_dep:
            for p, i in self.barrier_dep.items():
                add(p, i)
        return need

    def _note(self, pid, idx, R, W):
        for k in R:
            d = self.readers.setdefault(k, {})
            if d.get(pid, 0) < idx:
                d[pid] = idx
        for k in W:
            self.last_w[k] = (pid, idx)
            self.readers[k] = {}

    def _waits(self, stream, need, self_pid):
        waits = []
        for p, i in need.items():
            if p == self_pid and p == 'pe':
                continue
            if self.seen[stream].get(p, 0) >= i:
                continue
            self.seen[stream][p] = i
            waits.append((p, i))
            self.marked.add((p, i))
        return waits

    def op(self, eng, fn, R=(), W=()):
        need = self._deps(R, W)
        idx = self.prod.get(eng, 0) + 1
        self.prod[eng] = idx
        waits = self._waits(eng, need, eng)
        self.ops[eng].append(dict(fn=fn, waits=waits, pid=eng, idx=idx, dma=False))
        self._note(eng, idx, R, W)

    def dma(self, q, fn, R=(), W=()):
        lane = (q, self.lane_rr[q] % NLANE)
        self.lane_rr[q] += 1
        need = self._deps(R, W)
        idx = self.prod.get(lane, 0) + 1
        self.prod[lane] = idx
        if idx > 1:
            need[lane] = max(need.get(lane, 0), idx - 1)
        waits = self._waits(q, need, None)
        self.marked.add((lane, idx))
        self.ops[q].append(dict(fn=fn, waits=waits, pid=lane, idx=idx, dma=True))
        self._note(lane, idx, R, W)

    def barrier(self, fn_dve):
        need = dict(self.prod)
        self.barrier_dep = None
        idx = self.prod.get('dve', 0) + 1
        self.prod['dve'] = idx
        waits = self._waits('dve', need, 'dve')
        self.ops['dve'].append(dict(fn=fn_dve, waits=waits, pid='dve', idx=idx, dma=False))
        self.barrier_dep = {'dve': idx}

    def emit(self, nc, stack):
        ranks = {}
        by_p = {}
        for (p, i) in self.marked:
            by_p.setdefault(p, []).append(i)
        sems = {}
        for p, lst in by_p.items():
            lst.sort()
            for r, i in enumerate(lst):
                ranks[(p, i)] = r + 1
            nep = (len(lst) + EPOCH - 1) // EPOCH
            nm = p if isinstance(p, str) else f"{p[0]}{p[1]}"
            sems[p] = [stack.enter_context(nc.semaphore(f"s_{nm}_{k}")) for k in range(nep)]

        def semval(p, i):
            r = ranks[(p, i)]
            ep = (r - 1) // EPOCH
            v = r - ep * EPOCH
            return sems[p][ep], v * (1 if isinstance(p, str) else 16)

        def run(stream, eng):
            for o in self.ops[stream]:
                for (p, i) in o['waits']:
                    s, v = semval(p, i)
                    eng.wait_ge(s, v)
                if o['fn'] is None:
                    continue
                ins = o['fn'](eng)
                key = (o['pid'], o['idx'])
                if key in ranks:
                    s, _ = semval(*key)
                    ins.then_inc(s, 16 if o['dma'] else 1)

        block = stack.enter_context(nc.Block())

        @block.tensor
        def _(e):
            run('pe', e)

        @block.scalar
        def _(e):
            run('act', e)

        @block.vector
        def _(e):
            run('dve', e)

        @block.gpsimd
        def _(e):
            run('pool', e)

        @block.sync
        def _(e):
            run('sp', e)


class Arena:
    def __init__(self, nc, base, limit):
        self.nc, self.off, self.limit, self.n = nc, base, limit, 0

    def alloc(self, shape, dtype):
        size = int(np.prod(shape[1:])) * (2 if dtype == BF16 else 4)
        size = (size + 63) // 64 * 64
        self.n += 1
        t = self.nc.alloc_sbuf_tensor_at(f"t{self.n}_{self.off}", list(shape), dtype, offset=self.off)
        self.off += size
        assert self.off <= self.limit, (self.off, self.limit)
        return t


D = 1024
LAT = 2048
CTX = 256
NT = 18
NTOK = NT * 128
C = 32
NCH = 4
EPS = 1e-6
DFF = 2816
DEXP = 3584
NEXP = 8
BLKS = [(0, 256)] + [(256 + 512 * j, 512) for j in range(4)]
NCONST = 4 * 128 + 512
import os
DBG_NOROUTER = bool(os.environ.get('K_NOROUTER'))
DBG_SUB = os.environ.get('K_SUB', '')


def make_consts():
    j = np.arange(128)[:, None]
    i = np.arange(128)[None, :]
    same = (j // C) == (i // C)
    ident = np.eye(128, dtype=np.float32)
    triF = (same & (j <= i)).astype(np.float32)
    triB = (same & (j >= i)).astype(np.float32)
    ind = np.zeros((128, 128), np.float32)
    ind[np.arange(128), np.arange(128) // C] = 1.0
    mres = np.ones((128, 512), np.float32)
    mres[:, ::C] = 0.0
    return np.concatenate([ident, triF, triB, ind, mres], axis=1)


def build(n_layers=2, stop=None, nseq=2):
    nc = bass.Bass("TRN2", target_bir_lowering=False)

    def din(name, shape):
        return nc.dram_tensor(name, list(shape), F32, kind="ExternalInput").ap()
    x2 = din("x2", [2, LAT, D])
    ctx2 = din("ctx2", [2, CTX, D])
    c3T = din("c3T", [128, 24])
    cst = din("cst", [128, NCONST])
    w_mod = din("w_mod", [2, D, 6 * D])
    b_mod = din("b_mod", [2, 6 * D])
    norm1_g = din("norm1_g", [2, D])
    norm2_g = din("norm2_g", [2, D])
    w_in = din("w_in", [2, D, 4128])
    gate_w2 = din("gla_gate_w2", [2, 2, 16, 256])
    gate_b = din("gla_gate_b", [2, 2, 256])
    gla_ng = din("gla_norm_g", [2, 128])
    hg_ng = din("hgrn_norm_g", [2, 128])
    hgrn_lb = din("hgrn_lb", [2, 2, 512])
    w_out = din("w_out", [2, D, D])
    ffn_w1 = din("ffn_w1", [1, D, DFF])
    ffn_w3 = din("ffn_w3", [1, D, DFF])
    ffn_w2 = din("ffn_w2", [1, DFF, D])
    router = din("moe_router", [1, D, NEXP])
    moe_w1 = din("moe_w1", [1, NEXP, D, DEXP])
    moe_w3 = din("moe_w3", [1, NEXP, D, DEXP])
    moe_w2 = din("moe_w2", [1, NEXP, DEXP, D])
    fin_g = din("final_norm_g", [D])
    out2 = nc.dram_tensor("out2", [2, LAT, D], F32, kind="ExternalOutput").ap()
    dbg = None
    if stop is not None:
        dbg = nc.dram_tensor("dbg", [2, NTOK, D], F32, kind="ExternalOutput").ap()
    modsc = nc.dram_tensor("modsc", [2, 3, 6 * D], F32, kind="Internal").ap()
    xs = nc.dram_tensor("xs", [LAT, D], F32, kind="Internal").ap()

    S = Sched()
    with ExitStack() as st:
        base = (nc.SBUF_PARTITION_SIZE_BYTES - nc.sbuf_bytes_remaining + 63) // 64 * 64
        LIMIT = nc.SBUF_PARTITION_SIZE_BYTES
        A0 = Arena(nc, base, LIMIT)
        ps = [st.enter_context(nc.psum_tensor(f"ps{i}", [128, 512], F32)) for i in range(8)]
        psb = [p[:].bitcast(BF16) for p in ps]

        cf = A0.alloc([128, NCONST], F32)
        cb = A0.alloc([128, NCONST], BF16)
        identf = cf[:, 0:128]
        identb = cb[:, 0:128]
        maskF = cb[:, 128:256]
        maskB = cb[:, 256:384]
        indf = cf[:, 384:512]
        indb = cb[:, 384:512]
        mresb = cf[:, 512:1024]
        lball = A0.alloc([128, 16], F32)
        omlall = A0.alloc([128, 16], F32)
        ngcol = A0.alloc([128, 4], F32)
        small = A0.alloc([128, 64], F32)
        junk = A0.alloc([128, 1024], BF16)
        gates = A0.alloc([128, NT, NEXP], F32)
        hT = A0.alloc([128, 8, NTOK], BF16)
        base_regions = A0.off

        def eng_op(eng, name, R, W, **kw):
            S.op(eng, lambda e: getattr(e, name)(**kw), R, W)

        def ACT(out, in_, func, R, W, **kw):
            S.op('act', lambda e: e.activation(out=out, in_=in_, func=func, **kw), R, W)

        def MM(out, lhsT, rhs, R, W, start=True, stop=True, tp=None):
            if tp is None:
                S.op('pe', lambda e: e.matmul(out, lhsT=lhsT, rhs=rhs, start=start, stop=stop), R, W)
            else:
                S.op('pe', lambda e: e.matmul(out, lhsT=lhsT, rhs=rhs, start=start, stop=stop, tile_position=tp), R, W)

        def TR(out, in_, ident, R, W):
            S.op('pe', lambda e: e.transpose(out=out, in_=in_, identity=ident), R, W)

        def TT(eng, out, in0, in1, op, R, W):
            S.op(eng, lambda e: e.tensor_tensor(out=out, in0=in0, in1=in1, op=op), R, W)

        def TS(eng, out, in0, s1, s2, op0, op1, R, W):
            if s2 is None:
                S.op(eng, lambda e: e.tensor_scalar(out=out, in0=in0, scalar1=s1, scalar2=None, op0=op0), R, W)
            else:
                S.op(eng, lambda e: e.tensor_scalar(out=out, in0=in0, scalar1=s1, scalar2=s2, op0=op0, op1=op1), R, W)

        def STT(out, in0, scalar, in1, op0, op1, R, W):
            S.op('dve', lambda e: e.scalar_tensor_tensor(out=out, in0=in0, scalar=scalar, in1=in1, op0=op0, op1=op1), R, W)

        def DMA(q, out, in_, R, W, nc_ok=False):
            if nc_ok:
                S.dma(q, lambda e: e.dma_start(out=out, in_=in_, allow_slow_non_contiguous=True), R, W)
            else:
                S.dma(q, lambda e: e.dma_start(out=out, in_=in_), R, W)

        def rstd(ss, key):
            ACT(ss, ss, AF.Ln, [key], [key], bias=EPS)
            ACT(ss, ss, AF.Exp, [key], [key], scale=-0.5)

        DMA('sp', cf[:], cst, [], ['cf'])
        S.op('dve', lambda e: e.tensor_copy(out=cb[:], in_=cf[:]), ['cf'], ['cb'])
        CONST = ['cf', 'cb']
        DMA('sp', ngcol[:, 0:2], gla_ng.rearrange("l p -> p l"), [], ['ngcol'], nc_ok=True)
        DMA('sp', ngcol[:, 2:4], hg_ng.rearrange("l p -> p l"), [], ['ngcol'], nc_ok=True)
        DMA('sp', lball[:], hgrn_lb.rearrange("l d (h p) -> p (l d h)", p=128), [], ['lball'], nc_ok=True)
        e01 = small[:, 0:16]
        ACT(e01, lball[:], AF.Exp, ['lball'], ['e01'])
        ssum = small[:, 16:24]
        TT('dve', ssum, small[:, 0:8], small[:, 8:16], ALU.add, ['e01'], ['ssum'])
        S.op('dve', lambda e: e.reciprocal(out=ssum, in_=ssum), ['ssum'], ['ssum'])
        p0 = small[:, 24:32]
        p1 = small[:, 32:40]
        TT('dve', p0, small[:, 0:8], ssum, ALU.mult, ['e01', 'ssum'], ['p0'])
        TT('dve', p1, small[:, 8:16], ssum, ALU.mult, ['e01', 'ssum'], ['p1'])
        cum1 = small[:, 40:48]
        TT('dve', cum1, p0, p1, ALU.add, ['p0', 'p1'], ['cum1'])
        TT('dve', lball[:, 0:8], p0, p0, ALU.subtract, ['p0'], ['lball'])
        TT('dve', lball[:, 8:16], cum1, p0, ALU.subtract, ['cum1', 'p0'], ['lball'])
        TS('dve', lball[:], lball[:], 0.0, None, ALU.max, None, ['lball'], ['lball'])
        TS('dve', omlall[:], lball[:], -1.0, 1.0, ALU.mult, ALU.add, ['lball'], ['omlall'])

        Ap = Arena(nc, base_regions, LIMIT)
        ct = Ap.alloc([128, 24], F32)
        cs = Ap.alloc([128, 24], BF16)
        modv = Ap.alloc([3, 6 * D], F32)
        bm3 = Ap.alloc([3, 6 * D], F32)
        g13 = Ap.alloc([3, D], F32)
        g23 = Ap.alloc([3, D], F32)
        wm = [Ap.alloc([128, 8, 512], BF16) for _ in range(2)]
        DMA('sp', ct[:], c3T, [], ['ct'])
        ACT(cs[:], ct[:], AF.Silu, ['ct'], ['cs'])
        for l in range(n_layers):
            DMA('sp', bm3[:], b_mod[l, :].partition_broadcast(3), [], ['bm3'])
            DMA('sp', g13[:], norm1_g[l, :].partition_broadcast(3), [], ['g13'])
            DMA('sp', g23[:], norm2_g[l, :].partition_broadcast(3), [], ['g23'])
            for n in range(12):
                sl = n % 2
                DMA('pool', wm[sl][:], w_mod[l].rearrange("(k p) n -> p k n", p=128)[:, :, n * 512:(n + 1) * 512],
                    [], [f'wm{sl}'])
                for k in range(8):
                    MM(ps[sl][0:3, :], cs[:, 3 * k:3 * k + 3], wm[sl][:, k, :], ['cs', f'wm{sl}'], [f'ps{sl}'],
                       start=(k == 0), stop=(k == 7))
                TT('dve', modv[:, n * 512:(n + 1) * 512], ps[sl][0:3, :], bm3[:, n * 512:(n + 1) * 512], ALU.add,
                   [f'ps{sl}', 'bm3'], ['modv'])
            STT(modv[:, D:2 * D], modv[:, D:2 * D], 1.0, g13[:], ALU.add, ALU.mult, ['modv', 'g13'], ['modv'])
            STT(modv[:, 4 * D:5 * D], modv[:, 4 * D:5 * D], 1.0, g23[:], ALU.add, ALU.mult, ['modv', 'g23'], ['modv'])
            DMA('sp', modsc[l], modv[:], ['modv'], [f'modsc{l}'])
        S.barrier(lambda e: e.memset(small[0:1, 60:61], 0.0))

        def modvec(l, row, j):
            return modsc[l, row, j * D:(j + 1) * D].partition_broadcast(128)

        RA = base_regions
        acc = nc.alloc_sbuf_tensor_at("acc", [128, NT, D], F32, offset=RA)
        RB = RA + NT * D * 4
        RB_SIZE = 57344
        RC = RB + RB_SIZE

        def x_src(s, l, t):
            if l == 0:
                return ctx2[s, t * 128:(t + 1) * 128, :] if t < 2 else x2[s, (t - 2) * 128:(t - 1) * 128, :]
            return None

        def tile_cols(t):
            return slice(t * 128, (t + 1) * 128)

        for s in range(nseq):
            for l in range(n_layers):
                last = (l == n_layers - 1) and (l == 1)
                Ac = Arena(nc, RC, LIMIT)
                bcG = Ac.alloc([128, D], F32)
                bcS = Ac.alloc([128, D], F32)
                xt = [Ac.alloc([128, D], F32) for _ in range(2)]
                hfs = [Ac.alloc([128, D], F32) for _ in range(2)]
                ssq = Ac.alloc([128, 2], F32)
                for t in range(NT):
                    if t == 0 or t == 2:
                        row = 2 if t == 0 else s
                        DMA('sp', bcS[:], modvec(l, row, 0), [f'modsc{l}'], ['bcS'])
                        DMA('sp', bcG[:], modvec(l, row, 1), [f'modsc{l}'], ['bcG'])
                    if l == 0:
                        xa = xt[t % 2]
                        xk = f'xt{t % 2}'
                        DMA('sp', xa[:], x_src(s, l, t), [], [xk])
                        xin = xa[:]
                    else:
                        xin = acc[:, t, :]
                        xk = f'acc{t}'
                        if t >= 2:
                            DMA('sp', xs[(t - 2) * 128:(t - 1) * 128, :], acc[:, t, :], [xk], [f'xs{t}'])
                    hf = hfs[t % 2]
                    hfk = f'hf{t % 2}'
                    sq = ssq[:, t % 2:t % 2 + 1]
                    sqk = f'ssq{t % 2}'
                    ACT(junk[:], xin, AF.Square, [xk], [sqk], scale=D ** -0.5, accum_out=sq)
                    rstd(sq, sqk)
                    STT(hf[:], xin, sq, bcG[:], ALU.mult, ALU.mult, [xk, sqk, 'bcG'], [hfk])
                    TT('pool', hf[:], hf[:], bcS[:], ALU.add, [hfk, 'bcS'], [hfk])
                    for k in range(8):
                        TR(ps[(k // 4)][:, (k % 4) * 128:(k % 4 + 1) * 128], hf[:, k * 128:(k + 1) * 128], identf,
                           [hfk] + CONST, [f'ps{k // 4}'])
                    for hh in range(2):
                        ACT(hT[:, 4 * hh:4 * hh + 4, tile_cols(t)], ps[hh][:].rearrange("p (k c) -> p k c", c=128), AF.Copy,
                            [f'ps{hh}'], [f'hT{t}'])
                S.barrier(lambda e: e.memset(small[0:1, 60:61], 0.0))

                mixT = nc.alloc_sbuf_tensor_at(f"mixT_{s}_{l}", [128, 8, NTOK], BF16, offset=RB)
                wout = nc.alloc_sbuf_tensor_at(f"wout_{s}_{l}", [128, 8, D], BF16, offset=RB + 8 * NTOK * 2)
                Aa = Arena(nc, RA, RB)
                Ac = Arena(nc, RC, LIMIT)
                sgT = Aa.alloc([128, NTOK], BF16)
                qtT = [Aa.alloc([128, NTOK], BF16) for _ in range(2)]
                ktT = [Aa.alloc([128, NTOK], BF16) for _ in range(2)]
                kdec = [Aa.alloc([128, NT, 128], BF16) for _ in range(2)]
                vbf = Aa.alloc([128, NT, 128], BF16)
                o_fb = [Aa.alloc([128, NT, 128], F32) for _ in range(2)]
                a_all = [Aa.alloc([128, NT * NCH], F32) for _ in range(2)]
                win = [Aa.alloc([128, 8, 640], BF16)]
                w2a = [Aa.alloc([32, 256], BF16) for _ in range(2)]
                Sbf = [Aa.alloc([128, NCH, 128], BF16) for _ in range(2)]
                vblk = [Aa.alloc([128, NCH, 128], BF16) for _ in range(2)]
                PT = [Aa.alloc([128, 128], BF16) for _ in range(2)]
                tmpP = nc.alloc_sbuf_tensor_at(f"tmpP_{s}_{l}", [128, LAT], BF16, offset=Ac.off)
                qf = Ac.alloc([128, 512], F32)
                kf = [Ac.alloc([128, 512], F32) for _ in range(2)]
                uu = [Ac.alloc([128, 512], F32) for _ in range(2)]
                bF = Ac.alloc([128, 512], F32)
                bb = Ac.alloc([128, 512], F32)
                ee = Ac.alloc([128, 512], F32)
                rr = ee
                e2 = Ac.alloc([128, 512], F32)
                t1 = Ac.alloc([128, 512], F32)
                vTb = Ac.alloc([128, 512], BF16)
                kdT = Ac.alloc([128, 512], BF16)
                otot = [Ac.alloc([128, 128], F32) for _ in range(2)]
                onb = [Ac.alloc([128, 128], BF16) for _ in range(2)]
                wgr = Aa.alloc([128, 8, 32], BF16)
                Sst = [[Aa.alloc([128, 128], F32) for _ in range(2)], [Ac.alloc([128, 128], F32) for _ in range(2)]]
                oss = Ac.alloc([128, 2], F32)
                gfa = [Ac.alloc([32, NTOK], BF16) for _ in range(2)]

                DMA('pool', wout[:], w_out[l].rearrange("(k p) n -> p k n", p=128), [], ['wout'])
                winl = w_in[l].rearrange("(k p) n -> p k n", p=128)
                DMA('pool', wgr[:], winl[:, :, 1536:1568], [], ['wgr'])
                for d in range(2):
                    eng_op('pool', 'memset', [], [f'gfa{d}'], ap=gfa[d][:], constant=1.0)
                    DMA('pool', w2a[d][0:16, :], gate_w2[l, d], [], [f'w2a{d}'])
                    DMA('pool', w2a[d][16:17, :], gate_b[l, d:d + 1, :], [], [f'w2a{d}'])
                for bi, (b0, bn) in enumerate(BLKS):
                    for d in range(2):
                        for k in range(8):
                            MM(ps[d][0:16, 0:bn], wgr[:, k, 16 * d:16 * d + 16], hT[:, k, b0:b0 + bn],
                               ['wgr'] + [f'hT{t}' for t in range(b0 // 128, (b0 + bn) // 128)], [f'ps{d}'],
                               start=(k == 0), stop=(k == 7))
                        ACT(gfa[d][0:16, b0:b0 + bn], ps[d][0:16, 0:bn], AF.Copy, [f'ps{d}'], [f'gfa{d}'])

                def hblk(k, cm, bi):
                    b0, bn = BLKS[bi]
                    return hT[:, k, b0:b0 + bn]

                ALLH = [f'hT{t}' for t in range(NT)]
                for hd in range(8):
                    gla = hd < 4
                    if hd == 4:
                        LATK = [f'hT{t}' for t in range(2, NT)]
                        for k in range(8):
                            ACT(tmpP[:].rearrange("p (w r) -> p w r", w=64), hT[:, k, 256:NTOK].rearrange("p (r w) -> p w r", w=64), AF.Copy,
                                LATK, ['qf', 'kf0'])
                            eng_op('pool', 'tensor_copy', ['qf', 'kf0'], LATK, out=hT[:, k, 256:NTOK], in_=tmpP[:])
                    h = hd % 4
                    dk = 64 if gla else 128
                    gs = -1.0 / 16 if gla else -1.0
                    wslot = 0
                    wt = win[wslot]
                    wk = f'win{wslot}'
                    def emit_win(hx):
                        hh_ = hx % 4
                        if hx < 4:
                            pieces = [(64 * hh_, 64, 0), (256 + 64 * hh_, 64, 64), (512 + 128 * hh_, 128, 128), (1024 + 128 * hh_, 128, 256)]
                        else:
                            pieces = [(1568 + 128 * hh_, 128, 0), (2080 + 128 * hh_, 128, 128), (2592 + 128 * hh_, 128, 256),
                                      (3104 + 128 * hh_, 128, 384), (3616 + 128 * hh_, 128, 512)]
                        for (c0, w, o) in pieces:
                            DMA('pool', win[0][:, :, o:o + w], winl[:, :, c0:c0 + w], [], ['win0'])
                    if hd == 0:
                        emit_win(0)
                    ngc = ngcol[:, l:l + 1] if gla else ngcol[:, 2 + l:3 + l]

                    for bi, (b0, bn) in enumerate(BLKS):
                        t0 = b0 // 128
                        ntl = bn // 128

                        def emit_proj(bj):
                            pb0, pbn = BLKS[bj]

                            def proj(bank, M, co):
                                for k in range(8):
                                    MM(ps[bank][0:M, 0:pbn], wt[:, k, co:co + M], hblk(k, not gla, bj), [wk] + ALLH, [f'ps{bank}'],
                                       start=(k == 0), stop=(k == 7))
                            if gla:
                                proj(0, 64, 0)
                                proj(1, 64, 64)
                                proj(2, 128, 128)
                                proj(3, 128, 256)
                                for d in range(2):
                                    MM(ps[4 + d][0:64, 0:pbn], w2a[d][0:17, 64 * h:64 * h + 64], gfa[d][0:17, pb0:pb0 + pbn],
                                       [f'w2a{d}', f'gfa{d}'], [f'ps{4 + d}'])
                            else:
                                proj(0, 128, 0)
                                proj(1, 128, 128)
                                proj(4, 128, 256)
                                proj(2, 128, 384)
                                proj(3, 128, 512)
                        if bi == 0:
                            emit_proj(0)
                        if gla:
                            TS('dve', qf[0:64, 0:bn], ps[0][0:64, 0:bn], 0.125, None, ALU.mult, None, ['ps0'], ['qf'])
                            eng_op('dve', 'tensor_copy', ['ps1'], ['kf0'], out=kf[0][0:64, 0:bn], in_=ps[1][0:64, 0:bn])
                            kfd = [kf[0], kf[0]]
                            kfk = ['kf0', 'kf0']
                            for d in range(2):
                                ACT(ee[0:64, 0:bn], ps[4 + d][0:64, 0:bn], AF.Exp, [f'ps{4 + d}'], ['ee'], scale=-1.0)
                                ACT(uu[d][0:64, 0:bn], ee[0:64, 0:bn], AF.Ln, ['ee'], [f'uu{d}'], bias=1.0)
                        else:
                            ACT(qf[:, 0:bn], ps[0][:, 0:bn], AF.Silu, ['ps0'], ['qf'])
                            kfd = kf
                            kfk = ['kf0', 'kf1']
                            for d in range(2):
                                bank = 1 if d == 0 else 4
                                col = l * 8 + d * 4 + h
                                ACT(ee[:, 0:bn], ps[bank][:, 0:bn], AF.Exp, [f'ps{bank}'], ['ee'], scale=-1.0)
                                ACT(bb[:, 0:bn], ee[:, 0:bn], AF.Ln, ['ee'], ['bb'], bias=1.0)
                                ACT(t1[:, 0:bn], ee[:, 0:bn], AF.Ln, ['ee', 'lball'], ['t1'], bias=1.0, scale=lball[:, col:col + 1])
                                TT('pool', uu[d][:, 0:bn], bb[:, 0:bn], t1[:, 0:bn], ALU.subtract, ['bb', 't1'], [f'uu{d}'])
                                ACT(e2[:, 0:bn], bb[:, 0:bn], AF.Exp, ['bb'], ['e2'], scale=-1.0)
                                STT(kf[d][:, 0:bn], ee[:, 0:bn], omlall[:, col:col + 1], e2[:, 0:bn], ALU.mult, ALU.mult,
                                    ['ee', 'e2', 'omlall'], [f'kf{d}'])
                        ACT(vTb[:, 0:bn], ps[2][:, 0:bn], AF.Copy, ['ps2'], ['vTb'])
                        ACT(sgT[:, b0:b0 + bn], ps[3][:, 0:bn], AF.Silu, ['ps3'], [f'sgT{bi}'])
                        if bi + 1 < len(BLKS):
                            emit_proj(bi + 1)
                        for j in range(ntl):
                            TR(psb[6][:, j * 128:(j + 1) * 128], vTb[:, j * 128:(j + 1) * 128], identb, ['vTb'] + CONST, ['ps6'])
                        eng_op('dve', 'tensor_copy', ['ps6'], [f'vbf{t}' for t in range(t0, t0 + ntl)],
                               out=vbf[:, t0:t0 + ntl, :], in_=psb[6][:, 0:bn].rearrange("p (j c) -> p j c", c=128))
                        for d in range(2):
                            nchb = bn // C
                            eng_op('dve', 'tensor_tensor_scan', [f'uu{d}'] + CONST, ['bF'], out=bF[0:dk, 0:bn], data0=mresb[0:dk, 0:bn],
                                   data1=uu[d][0:dk, 0:bn], initial=0.0, op0=ALU.mult, op1=ALU.add)
                            blast = bF[0:dk, 0:bn].rearrange("p (c j) -> p c j", j=C)[:, :, C - 1:C]
                            bl_bc = blast.to_broadcast([dk, nchb, C])
                            bF3 = bF[0:dk, 0:bn].rearrange("p (c j) -> p c j", j=C)
                            rr3 = rr[0:dk, 0:bn].rearrange("p (c j) -> p c j", j=C)
                            if d == 0:
                                bsrc = bF
                                bkey = 'bF'
                                TT('dve', rr3, bl_bc, bF3, ALU.subtract, ['bF'], ['ee'])
                            else:
                                TT('dve', rr[0:dk, 0:bn], bF[0:dk, 0:bn], uu[d][0:dk, 0:bn], ALU.subtract, ['bF', f'uu{d}'], ['ee'])
                                bb3 = bb[0:dk, 0:bn].rearrange("p (c j) -> p c j", j=C)
                                TT('pool', bb3, bl_bc, rr3, ALU.subtract, ['bF', 'ee'], ['bb'])
                                bsrc = bb
                                bkey = 'bb'
                            ACT(a_all[d][0:dk, (b0 // C):(b0 // C) + nchb], bF[0:dk, 0:bn].rearrange("p (c j) -> p c j", j=C)[:, :, C - 1],
                                AF.Exp, ['bF'], [f'a{d}'], scale=gs)
                            ACT(e2[0:dk, 0:bn], bsrc[0:dk, 0:bn], AF.Exp, [bkey], ['e2'], scale=gs)
                            TT('dve', qtT[d][0:dk, b0:b0 + bn], qf[0:dk, 0:bn], e2[0:dk, 0:bn], ALU.mult, ['qf', 'e2'], [f'qtT{d}_{bi}'])
                            ACT(t1[0:dk, 0:bn], bsrc[0:dk, 0:bn], AF.Exp, [bkey], ['t1'], scale=-gs)
                            TT('dve', ktT[d][0:dk, b0:b0 + bn], kfd[d][0:dk, 0:bn], t1[0:dk, 0:bn], ALU.mult, [kfk[d], 't1'], [f'ktT{d}_{bi}'])
                            ACT(e2[0:dk, 0:bn], rr[0:dk, 0:bn], AF.Exp, ['ee'], ['e2'], scale=gs)
                            TT('pool', kdT[0:dk, 0:bn], kfd[d][0:dk, 0:bn], e2[0:dk, 0:bn], ALU.mult, [kfk[d], 'e2'], ['kdT'])
                            for j in range(ntl):
                                TR(psb[7][:, j * 128:j * 128 + dk], kdT[0:dk, j * 128:(j + 1) * 128], identb[0:dk, 0:dk], ['kdT'] + CONST, ['ps7'])
                            ACT(kdec[d][:, t0:t0 + ntl, 0:dk], psb[7][:, 0:bn].rearrange("p (j c) -> p j c", c=128)[:, :, 0:dk], AF.Copy,
                                ['ps7'], [f'kdec{d}_{t}' for t in range(t0, t0 + ntl)])

                    if hd + 1 < 8:
                        emit_win(hd + 1)
                    orders = [list(range(NT)), [1, 0] + list(range(NT - 1, 1, -1))]
                    corders = [list(range(NCH)), list(range(NCH - 1, -1, -1))]
                    masks = [maskF, maskB]
                    PKV = [5, 6]
                    PSC = [0, 3]
                    PO = [1, 4]
                    for d in range(2):
                        eng_op('pool', 'memset', [], [f'Sst{d}_0'], ap=Sst[d][0][:], constant=0.0)
                        eng_op('pool', 'memset', [], [f'Sbf{d}_0'], ap=Sbf[d][:, 0, :], constant=0.0)
                    sp = [0, 0]
                    for it in range(NT):
                        tt = [orders[d][it] for d in range(2)]
                        need = [not (last and tt[d] < 2) for d in range(2)]
                        bis = [0 if tt[d] < 2 else 1 + (tt[d] - 2) // 4 for d in range(2)]
                        for d in range(2):
                            t = tt[d]
                            TT('dve', vblk[d][:], vbf[:, t, :].unsqueeze(1).to_broadcast([128, NCH, 128]),
                               indf[:, 0:NCH].unsqueeze(2).to_broadcast([128, NCH, 128]), ALU.mult, [f'vbf{t}'] + CONST, [f'vblk{d}'])
                            MM(ps[PKV[d]][0:dk, :], kdec[d][:, t, 0:dk], vblk[d][:].rearrange("p c e -> p (c e)"), [f'kdec{d}_{t}', f'vblk{d}'], [f'ps{PKV[d]}'])
                        for d in range(2):
                            t = tt[d]
                            if need[d]:
                                MM(ps[PSC[d]][:, 0:128], ktT[d][0:dk, tile_cols(t)], qtT[d][0:dk, tile_cols(t)],
                                   [f'ktT{d}_{bis[d]}', f'qtT{d}_{bis[d]}'], [f'ps{PSC[d]}'])
                        for d in range(2):
                            t = tt[d]
                            if need[d]:
                                TT('dve', PT[d][:], ps[PSC[d]][:, 0:128], masks[d], ALU.mult, [f'ps{PSC[d]}'] + CONST, [f'PT{d}'])
                                MM(ps[PO[d]][:, 0:128], PT[d][:], vbf[:, t, :], [f'PT{d}', f'vbf{t}'], [f'ps{PO[d]}'], start=True, stop=False)
                        for ci in range(NCH):
                            for d in range(2):
                                t = tt[d]
                                c = corders[d][ci]
                                if need[d]:
                                    MM(ps[PO[d]][C * c:C * c + C, 0:128], qtT[d][0:dk, t * 128 + C * c:t * 128 + C * c + C], Sbf[d][0:dk, ci, :],
                                       [f'qtT{d}_{bis[d]}', f'Sbf{d}_{ci}'], [f'ps{PO[d]}'], start=False, stop=True, tp=(0, C * c))
                            for d in range(2):
                                t = tt[d]
                                c = corders[d][ci]
                                p = sp[d]
                                STT(Sst[d][1 - p][0:dk, :], Sst[d][p][0:dk, :], a_all[d][0:dk, t * NCH + c:t * NCH + c + 1],
                                    ps[PKV[d]][0:dk, c * 128:(c + 1) * 128], ALU.mult, ALU.add, [f'Sst{d}_{p}', f'a{d}', f'ps{PKV[d]}'], [f'Sst{d}_{1 - p}'])
                                sp[d] = 1 - p
                            for d in range(2):
                                nslot = (ci + 1) if ci < NCH - 1 else 0
                                ACT(Sbf[d][0:dk, nslot, :], Sst[d][sp[d]][0:dk, :], AF.Copy, [f'Sst{d}_{sp[d]}'], [f'Sbf{d}_{nslot}'])
                        for d in range(2):
                            if need[d]:
                                ACT(o_fb[d][:, tt[d], :], ps[PO[d]][:, 0:128], AF.Copy, [f'ps{PO[d]}'], [f'o{d}_{tt[d]}'])
                    for t in range(NT):
                        if last and t < 2:
                            continue
                        bi = 0 if t < 2 else 1 + (t - 2) // 4
                        es = t % 2
                        epb = 2 if es == 0 else 7
                        TT('pool', otot[es][:], o_fb[0][:, t, :], o_fb[1][:, t, :], ALU.add, [f'o0_{t}', f'o1_{t}'], [f'otot{es}'])
                        ACT(junk[:, 0:128], otot[es][:], AF.Square, [f'otot{es}'], [f'oss{es}'], scale=128 ** -0.5, accum_out=oss[:, es:es + 1])
                        rstd(oss[:, es:es + 1], f'oss{es}')
                        TS('dve', onb[es][:], otot[es][:], oss[:, es:es + 1], None, ALU.mult, None, [f'otot{es}', f'oss{es}'], [f'onb{es}'])
                        TR(psb[epb][:, 0:128], onb[es][:], identb, [f'onb{es}'] + CONST, [f'ps{epb}'])
                        if gla or t < 2:
                            STT(mixT[:, hd, tile_cols(t)], psb[epb][:, 0:128], ngc, sgT[:, tile_cols(t)], ALU.mult, ALU.mult,
                                [f'ps{epb}', f'sgT{bi}', 'ngcol'], [f'mixT{hd}'])
                        else:
                            j = t - 2
                            ov = mixT[:, hd, 256:NTOK].rearrange("p (r w) -> p w r", w=64)[:, 4 * j:4 * j + 4, :]
                            STT(ov, psb[epb][:, 0:128].rearrange("p (w r) -> p w r", w=4), ngc,
                                sgT[:, tile_cols(t)].rearrange("p (w r) -> p w r", w=4), ALU.mult, ALU.mult,
                                [f'ps{epb}', f'sgT{bi}', 'ngcol'], [f'mixT{hd}'])
                S.barrier(lambda e: e.memset(small[0:1, 60:61], 0.0))

                Ac = Arena(nc, RC, LIMIT)
                bcG1 = Ac.alloc([128, D], F32)
                bcG2 = Ac.alloc([128, D], F32)
                bcS2 = Ac.alloc([128, D], F32)
                xt = [Ac.alloc([128, D], F32) for _ in range(2)]
                Bb = [Ac.alloc([128, D], F32) for _ in range(2)]
                hTf = Ac.alloc([128, 8, 128], F32)
                rtf = Ac.alloc([128, 8, NEXP], F32)
                lg = Ac.alloc([128, 4 * NEXP], F32)
                ssq = Ac.alloc([128, 8], F32)
                moe = (l % 2 == 1)
                if moe:
                    DMA('sp', rtf[:], router[0].rearrange("(k p) e -> p k e", p=128), [], ['rtf'], nc_ok=True)
                MIXK = [f'mixT{hd}' for hd in range(8)]
                for t in range(NT):
                    if last and t < 2:
                        continue
                    if t == 0 or t == 2:
                        row = 2 if t == 0 else s
                        DMA('sp', bcG1[:], modvec(l, row, 2), [f'modsc{l}'], ['bcG1'])
                        DMA('sp', bcS2[:], modvec(l, row, 3), [f'modsc{l}'], ['bcS2'])
                        DMA('sp', bcG2[:], modvec(l, row, 4), [f'modsc{l}'], ['bcG2'])
                    tmp = hf = Bb[t % 2]
                    bk = f'B{t % 2}'
                    sq = ssq[:, 7 * (t % 2):7 * (t % 2) + 1]
                    sqk = f'ssqn{t % 2}'
                    xa = xt[t % 2]
                    xk = f'xt{t % 2}'
                    if l == 0:
                        DMA('sp', xa[:], x_src(s, l, t), [], [xk])
                    else:
                        DMA('sp', xa[:], xs[(t - 2) * 128:(t - 1) * 128, :], [f'xs{t}'], [xk])
                    for nh in range(2):
                        for k in range(8):
                            MM(ps[nh][:, :], mixT[:, k, tile_cols(t)], wout[:, k, nh * 512:(nh + 1) * 512], MIXK + ['wout'], [f'ps{nh}'],
                               start=(k == 0), stop=(k == 7))
                        TT('dve', tmp[:, nh * 512:(nh + 1) * 512], ps[nh][:, :], bcG1[:, nh * 512:(nh + 1) * 512], ALU.mult,
                           [f'ps{nh}', 'bcG1'], [bk])
                    TT('pool', acc[:, t, :], xa[:], tmp[:], ALU.add, [xk, bk], [f'acc{t}'])
                    ak = f'acc{t}'
                    ACT(junk[:], acc[:, t, :], AF.Square, [ak], [sqk], scale=D ** -0.5, accum_out=sq)
                    rstd(sq, sqk)
                    STT(hf[:], acc[:, t, :], sq, bcG2[:], ALU.mult, ALU.mult, [ak, sqk, 'bcG2', bk], [bk])
                    TT('pool', hf[:], hf[:], bcS2[:], ALU.add, [bk, 'bcS2'], [bk])
                    for k in range(8):
                        TR(ps[2 + (k // 4)][:, (k % 4) * 128:(k % 4 + 1) * 128], hf[:, k * 128:(k + 1) * 128], identf,
                           [bk] + CONST, [f'ps{2 + k // 4}'])
                    for hh in range(2):
                        ACT(hT[:, 4 * hh:4 * hh + 4, tile_cols(t)], ps[2 + hh][:].rearrange("p (k c) -> p k c", c=128), AF.Copy,
                            [f'ps{2 + hh}'], [f'hT{t}'])
                    if moe and t >= 2 and not DBG_NOROUTER:
                        for hh in range(2):
                            ACT(hTf[:, 4 * hh:4 * hh + 4, :], ps[2 + hh][:].rearrange("p (k c) -> p k c", c=128), AF.Copy, [f'ps{2 + hh}'], ['hTf'])
                        if DBG_SUB == '1':
                            continue
                        for k in range(8):
                            MM(ps[4][:, 0:NEXP], hTf[:, k, :], rtf[:, k, :], ['hTf', 'rtf'], ['ps4'], start=(k == 0), stop=(k == 7))
                        if DBG_SUB == '2':
                            continue
                        L = lg[:, 0:8]
                        EQ1 = lg[:, 8:16]
                        L2 = lg[:, 16:24]
                        EQ2 = lg[:, 24:32]
                        eng_op('dve', 'tensor_copy', ['ps4'], ['lg'], out=L, in_=ps[4][:, 0:NEXP])
                        eng_op('dve', 'reduce_max', ['lg'], ['ssq'], out=ssq[:, 1:2], in_=L, axis=mybir.AxisListType.X)
                        TS('dve', EQ1, L, ssq[:, 1:2], None, ALU.is_equal, None, ['lg', 'ssq'], ['lg'])
                        STT(L2, EQ1, -1e30, L, ALU.mult, ALU.add, ['lg'], ['lg'])
                        eng_op('dve', 'reduce_max', ['lg'], ['ssq'], out=ssq[:, 2:3], in_=L2, axis=mybir.AxisListType.X)
                        TS('dve', EQ2, L2, ssq[:, 2:3], None, ALU.is_equal, None, ['lg', 'ssq'], ['lg'])
                        if DBG_SUB == '3':
                            continue
                        TT('dve', ssq[:, 3:4], ssq[:, 2:3], ssq[:, 1:2], ALU.subtract, ['ssq'], ['ssq'])
                        ACT(ssq[:, 4:5], ssq[:, 3:4], AF.Exp, ['ssq'], ['ssq'])
                        TS('dve', ssq[:, 5:6], ssq[:, 4:5], 1.0, None, ALU.add, None, ['ssq'], ['ssq'])
                        eng_op('dve', 'reciprocal', ['ssq'], ['ssq'], out=ssq[:, 5:6], in_=ssq[:, 5:6])
                        TT('dve', ssq[:, 6:7], ssq[:, 4:5], ssq[:, 5:6], ALU.mult, ['ssq'], ['ssq'])
                        TS('dve', gates[:, t, :], EQ1, ssq[:, 5:6], None, ALU.mult, None, ['lg', 'ssq'], [f'gates{t}'])
                        STT(gates[:, t, :], EQ2, ssq[:, 6:7], gates[:, t, :], ALU.mult, ALU.add, ['lg', 'ssq', f'gates{t}'], [f'gates{t}'])
                if stop == ('mix', l):
                    for t in range(NT):
                        DMA('sp', dbg[s, t * 128:(t + 1) * 128, :], acc[:, t, :], [f'acc{t}'], [f'dbg{s}_{t}'])
                    S.barrier(lambda e: e.memset(small[0:1, 60:61], 0.0))
                    break
                S.barrier(lambda e: e.memset(small[0:1, 60:61], 0.0))

                Ab = Arena(nc, RB, RC)
                GS = 4 if moe else 2
                w1g = [Ab.alloc([128, 8, GS * 128], BF16) for _ in range(2)]
                w3g = [Ab.alloc([128, 8, GS * 128], BF16) for _ in range(2)]
                w2g = [Ab.alloc([128, GS, D], BF16) for _ in range(2)]
                aT = [Ab.alloc([128, GS, 512], BF16) for _ in range(2)]
                Ac2 = Arena(nc, RC, LIMIT)
                s1 = [Ac2.alloc([128, 512], F32) for _ in range(2)]
                bcg = [Ac2.alloc([128, D], F32) for _ in range(2)]
                tm = [Ac2.alloc([128, D], F32) for _ in range(2)]
                DMA('sp', bcg[0][:], modvec(l, 2, 5), [f'modsc{l}'], ['bcg0'])
                DMA('sp', bcg[1][:], modvec(l, s, 5), [f'modsc{l}'], ['bcg1'])
                nE = NEXP if moe else 1
                FF = DEXP if moe else DFF
                ngroups = FF // (GS * 128)
                tblocks = ([] if last else [(0, 256)]) + [(256 + 512 * j, 512) for j in range(4)]
                groups = [(ex, g) for ex in range(nE) for g in range(ngroups)]

                def emit_wdma(gidx):
                    ex, g = groups[gidx]
                    sl = gidx % 2
                    W1 = moe_w1[0, ex] if moe else ffn_w1[0]
                    W3 = moe_w3[0, ex] if moe else ffn_w3[0]
                    W2 = moe_w2[0, ex] if moe else ffn_w2[0]
                    f0 = g * GS * 128
                    DMA('pool', w1g[sl][:], W1.rearrange("(k p) n -> p k n", p=128)[:, :, f0:f0 + GS * 128], [], [f'w1g{sl}'])
                    DMA('pool', w3g[sl][:], W3.rearrange("(k p) n -> p k n", p=128)[:, :, f0:f0 + GS * 128], [], [f'w3g{sl}'])
                    DMA('pool', w2g[sl][:], W2[f0:f0 + GS * 128, :].rearrange("(g p) n -> p g n", p=128), [], [f'w2g{sl}'])

                items = [(gidx, bidx) for gidx in range(len(groups)) for bidx in range(len(tblocks))]

                def stage1(ii):
                    gidx, bidx = items[ii]
                    sl = gidx % 2
                    asl = ii % 2
                    b0, bn = tblocks[bidx]
                    hk = [f'hT{t}' for t in range(b0 // 128, (b0 + bn) // 128)]
                    for f in range(GS):
                        pb = 2 * (f % 2)
                        for k in range(8):
                            MM(ps[pb][:, 0:bn], w1g[sl][:, k, f * 128:(f + 1) * 128], hT[:, k, b0:b0 + bn], [f'w1g{sl}'] + hk, [f'ps{pb}'],
                               start=(k == 0), stop=(k == 7))
                        for k in range(8):
                            MM(ps[pb + 1][:, 0:bn], w3g[sl][:, k, f * 128:(f + 1) * 128], hT[:, k, b0:b0 + bn], [f'w3g{sl}'] + hk, [f'ps{pb + 1}'],
                               start=(k == 0), stop=(k == 7))
                        ACT(s1[f % 2][:, 0:bn], ps[pb][:, 0:bn], AF.Silu, [f'ps{pb}'], [f's1{f % 2}'])
                        TT('dve', aT[asl][:, f, 0:bn], ps[pb + 1][:, 0:bn], s1[f % 2][:, 0:bn], ALU.mult,
                           [f'ps{pb + 1}', f's1{f % 2}'], [f'aT{asl}'])

                tcount = [0]

                def stage2(ii):
                    gidx, bidx = items[ii]
                    ex, g = groups[gidx]
                    sl = gidx % 2
                    asl = ii % 2
                    b0, bn = tblocks[bidx]
                    for j in range(bn // 128):
                        t = b0 // 128 + j
                        tsl = tcount[0] % 2
                        tcount[0] += 1
                        for nh in range(2):
                            pbk = 4 + 2 * tsl + nh
                            for f in range(GS):
                                MM(ps[pbk][:, :], aT[asl][:, f, j * 128:(j + 1) * 128], w2g[sl][:, f, nh * 512:(nh + 1) * 512],
                                   [f'aT{asl}', f'w2g{sl}'], [f'ps{pbk}'], start=(f == 0), stop=(f == GS - 1))
                            gsc = gates[:, t, ex:ex + 1] if moe else 1.0
                            bg = bcg[0 if t < 2 else 1]
                            STT(tm[tsl][:, nh * 512:(nh + 1) * 512], ps[pbk][:, :], gsc, bg[:, nh * 512:(nh + 1) * 512], ALU.mult, ALU.mult,
                                [f'ps{pbk}', f'gates{t}', 'bcg0', 'bcg1'], [f'tm{tsl}'])
                        TT('pool', acc[:, t, :], acc[:, t, :], tm[tsl][:], ALU.add, [f'acc{t}', f'tm{tsl}'], [f'acc{t}'])

                emit_wdma(0)
                if len(groups) > 1:
                    emit_wdma(1)
                stage1(0)
                for ii in range(len(items)):
                    if ii + 1 < len(items):
                        stage1(ii + 1)
                    stage2(ii)
                    gidx, bidx = items[ii]
                    if bidx == len(tblocks) - 1 and gidx + 2 < len(groups):
                        emit_wdma(gidx + 2)
                if stop == ('ffn', l):
                    for t in range(NT):
                        DMA('sp', dbg[s, t * 128:(t + 1) * 128, :], acc[:, t, :], [f'acc{t}'], [f'dbg{s}_{t}'])
                    S.barrier(lambda e: e.memset(small[0:1, 60:61], 0.0))
                    break
                S.barrier(lambda e: e.memset(small[0:1, 60:61], 0.0))
            else:
                Ac = Arena(nc, RC, LIMIT)
                fg = Ac.alloc([128, D], F32)
                ot = [Ac.alloc([128, D], F32) for _ in range(2)]
                ssq = Ac.alloc([128, 2], F32)
                DMA('sp', fg[:], fin_g.partition_broadcast(128), [], ['fg'])
                for t in range(2, NT):
                    ACT(junk[:], acc[:, t, :], AF.Square, [f'acc{t}'], ['ssq'], scale=D ** -0.5, accum_out=ssq[:, 0:1])
                    rstd(ssq[:, 0:1], 'ssq')
                    STT(ot[t % 2][:], acc[:, t, :], ssq[:, 0:1], fg[:], ALU.mult, ALU.mult, [f'acc{t}', 'ssq', 'fg'], [f'ot{t % 2}'])
                    DMA('sp', out2[s, (t - 2) * 128:(t - 1) * 128, :], ot[t % 2][:], [f'ot{t % 2}'], [f'out{s}_{t}'])
                S.barrier(lambda e: e.memset(small[0:1, 60:61], 0.0))
        S.op('sp', None, [k for k in S.last_w if isinstance(k, str) and (k.startswith('out') or k.startswith('dbg'))], [])
        S.emit(nc, st)
    return nc


_CACHE = {}


def make_in_maps(inputs, ncores=8):
    f = lambda a: np.ascontiguousarray(np.asarray(a, dtype=np.float32))
    cst = make_consts()
    shared = {k: f(inputs[k]) for k in ("w_mod", "b_mod", "norm1_g", "norm2_g", "w_in", "gla_gate_w2", "gla_gate_b",
                                        "gla_norm_g", "hgrn_norm_g", "hgrn_lb", "w_out", "ffn_w1", "ffn_w3", "ffn_w2",
                                        "moe_router", "moe_w1", "moe_w3", "moe_w2", "final_norm_g")}
    x, c, ctx, c_ctx = f(inputs["x"]), f(inputs["c"]), f(inputs["ctx"]), f(inputs["c_ctx"])
    maps = []
    for i in range(ncores):
        c3 = np.stack([c[2 * i], c[2 * i + 1], c_ctx], axis=0)
        c3T = np.ascontiguousarray(c3.reshape(3, 8, 128).transpose(2, 1, 0).reshape(128, 24))
        m = dict(shared)
        m.update(x2=np.ascontiguousarray(x[2 * i:2 * i + 2]), ctx2=np.ascontiguousarray(ctx[2 * i:2 * i + 2]), c3T=c3T, cst=cst)
        maps.append(m)
    return maps


def kernel(**inputs):
    if 'nc' not in _CACHE:
        _CACHE['nc'] = build()
    nc = _CACHE['nc']
    maps = make_in_maps(inputs)
    res = run_bass_kernel_spmd(nc, maps, core_ids=list(range(8)))
    return np.concatenate([r["out2"] for r in res.results], axis=0).astype(np.float32)
```

```python
from contextlib import ExitStack
import numpy as np
import concourse.bass as bass
import concourse.mybir as mybir
from concourse.bass_utils import run_bass_kernel_spmd

F32 = mybir.dt.float32
BF16 = mybir.dt.bfloat16
AF = mybir.ActivationFunctionType
ALU = mybir.AluOpType

CE = ('pe', 'act', 'dve', 'pool', 'sp')
EPOCH = 12000
NLANE = 8


class Sched:
    def __init__(self):
        self.ops = {e: [] for e in CE}
        self.prod = {}
        self.last_w = {}
        self.readers = {}
        self.seen = {e: {} for e in CE}
        self.lane_rr = {'sp': 0, 'pool': 0}
        self.marked = set()
        self.barrier_dep = None

    def _deps(self, R, W):
        need = {}

        def add(p, i):
            if need.get(p, 0) < i:
                need[p] = i
        for k in R:
            lw = self.last_w.get(k)
            if lw:
                add(*lw)
        for k in W:
            lw = self.last_w.get(k)
            if lw:
                add(*lw)
            for p, i in self.readers.get(k, {}).items():
                add(p, i)
        if self.barrier_dep:
            for p, i in self.barrier_dep.items():
                add(p, i)
        return need

    def _note(self, pid, idx, R, W):
        for k in R:
            d = self.readers.setdefault(k, {})
            if d.get(pid, 0) < idx:
                d[pid] = idx
        for k in W:
            self.last_w[k] = (pid, idx)
            self.readers[k] = {}

    def _waits(self, stream, need, self_pid):
        waits = []
        for p, i in need.items():
            if p == self_pid and p == 'pe':
                continue
            if self.seen[stream].get(p, 0) >= i:
                continue
            self.seen[stream][p] = i
            waits.append((p, i))
            self.marked.add((p, i))
        return waits

    def op(self, eng, fn, R=(), W=()):
        need = self._deps(R, W)
        idx = self.prod.get(eng, 0) + 1
        self.prod[eng] = idx
        waits = self._waits(eng, need, eng)
        self.ops[eng].append(dict(fn=fn, waits=waits, pid=eng, idx=idx, dma=False))
        self._note(eng, idx, R, W)

    def dma(self, q, fn, R=(), W=()):
        lane = (q, self.lane_rr[q] % NLANE)
        self.lane_rr[q] += 1
        need = self._deps(R, W)
        idx = self.prod.get(lane, 0) + 1
        self.prod[lane] = idx
        if idx > 1:
            need[lane] = max(need.get(lane, 0), idx - 1)
        waits = self._waits(q, need, None)
        self.marked.add((lane, idx))
        self.ops[q].append(dict(fn=fn, waits=waits, pid=lane, idx=idx, dma=True))
        self._note(lane, idx, R, W)

    def barrier(self, fn_dve):
        need = dict(self.prod)
        self.barrier_dep = None
        idx = self.prod.get('dve', 0) + 1
        self.prod['dve'] = idx
        waits = self._waits('dve', need, 'dve')
        self.ops['dve'].append(dict(fn=fn_dve, waits=waits, pid='dve', idx=idx, dma=False))
        self.barrier_dep = {'dve': idx}

    def emit(self, nc, stack):
        ranks = {}
        by_p = {}
        for (p, i) in self.marked:
            by_p.setdefault(p, []).append(i)
        sems = {}
        for p, lst in by_p.items():
            lst.sort()
            for r, i in enumerate(lst):
                ranks[(p, i)] = r + 1
            nep = (len(lst) + EPOCH - 1) // EPOCH
            nm = p if isinstance(p, str) else f"{p[0]}{p[1]}"
            sems[p] = [stack.enter_context(nc.semaphore(f"s_{nm}_{k}")) for k in range(nep)]

        def semval(p, i):
            r = ranks[(p, i)]
            ep = (r - 1) // EPOCH
            v = r - ep * EPOCH
            return sems[p][ep], v * (1 if isinstance(p, str) else 16)

        def run(stream, eng):
            for o in self.ops[stream]:
                for (p, i) in o['waits']:
                    s, v = semval(p, i)
                    eng.wait_ge(s, v)
                if o['fn'] is None:
                    continue
                ins = o['fn'](eng)
                key = (o['pid'], o['idx'])
                if key in ranks:
                    s, _ = semval(*key)
                    ins.then_inc(s, 16 if o['dma'] else 1)

        block = stack.enter_context(nc.Block())

        @block.tensor
        def _(e):
            run('pe', e)

        @block.scalar
        def _(e):
            run('act', e)

        @block.vector
        def _(e):
            run('dve', e)

        @block.gpsimd
        def _(e):
            run('pool', e)

        @block.sync
        def _(e):
            run('sp', e)


class Arena:
    def __init__(self, nc, base, limit):
        self.nc, self.off, self.limit, self.n = nc, base, limit, 0

    def alloc(self, shape, dtype):
        size = int(np.prod(shape[1:])) * (2 if dtype == BF16 else 4)
        size = (size + 63) // 64 * 64
        self.n += 1
        t = self.nc.alloc_sbuf_tensor_at(f"t{self.n}_{self.off}", list(shape), dtype, offset=self.off)
        self.off += size
        assert self.off <= self.limit, (self.off, self.limit)
        return t


D = 1024
LAT = 2048
CTX = 256
NT = 18
NTOK = NT * 128
C = 32
NCH = 4
EPS = 1e-6
DFF = 2816
DEXP = 3584
NEXP = 8
BLKS = [(0, 256)] + [(256 + 512 * j, 512) for j in range(4)]
NCONST = 4 * 128 + 512
import os
DBG_NOROUTER = bool(os.environ.get('K_NOROUTER'))
DBG_SUB = os.environ.get('K_SUB', '')


def make_consts():
    j = np.arange(128)[:, None]
    i = np.arange(128)[None, :]
    same = (j // C) == (i // C)
    ident = np.eye(128, dtype=np.float32)
    triF = (same & (j <= i)).astype(np.float32)
    triB = (same & (j >= i)).astype(np.float32)
    ind = np.zeros((128, 128), np.float32)
    ind[np.arange(128), np.arange(128) // C] = 1.0
    mres = np.ones((128, 512), np.float32)
    mres[:, ::C] = 0.0
    return np.concatenate([ident, triF, triB, ind, mres], axis=1)


def build(n_layers=2, stop=None, nseq=2):
    nc = bass.Bass("TRN2", target_bir_lowering=False)

    def din(name, shape):
        return nc.dram_tensor(name, list(shape), F32, kind="ExternalInput").ap()
    x2 = din("x2", [2, LAT, D])
    ctx2 = din("ctx2", [2, CTX, D])
    c3T = din("c3T", [128, 24])
    cst = din("cst", [128, NCONST])
    w_mod = din("w_mod", [2, D, 6 * D])
    b_mod = din("b_mod", [2, 6 * D])
    norm1_g = din("norm1_g", [2, D])
    norm2_g = din("norm2_g", [2, D])
    w_in = din("w_in", [2, D, 4128])
    gate_w2 = din("gla_gate_w2", [2, 2, 16, 256])
    gate_b = din("gla_gate_b", [2, 2, 256])
    gla_ng = din("gla_norm_g", [2, 128])
    hg_ng = din("hgrn_norm_g", [2, 128])
    hgrn_lb = din("hgrn_lb", [2, 2, 512])
    w_out = din("w_out", [2, D, D])
    ffn_w1 = din("ffn_w1", [1, D, DFF])
    ffn_w3 = din("ffn_w3", [1, D, DFF])
    ffn_w2 = din("ffn_w2", [1, DFF, D])
    router = din("moe_router", [1, D, NEXP])
    moe_w1 = din("moe_w1", [1, NEXP, D, DEXP])
    moe_w3 = din("moe_w3", [1, NEXP, D, DEXP])
    moe_w2 = din("moe_w2", [1, NEXP, DEXP, D])
    fin_g = din("final_norm_g", [D])
    out2 = nc.dram_tensor("out2", [2, LAT, D], F32, kind="ExternalOutput").ap()
    dbg = None
    if stop is not None:
        dbg = nc.dram_tensor("dbg", [2, NTOK, D], F32, kind="ExternalOutput").ap()
    modsc = nc.dram_tensor("modsc", [2, 3, 6 * D], F32, kind="Internal").ap()
    xs = nc.dram_tensor("xs", [LAT, D], F32, kind="Internal").ap()

    S = Sched()
    with ExitStack() as st:
        base = (nc.SBUF_PARTITION_SIZE_BYTES - nc.sbuf_bytes_remaining + 63) // 64 * 64
        LIMIT = nc.SBUF_PARTITION_SIZE_BYTES
        A0 = Arena(nc, base, LIMIT)
        ps = [st.enter_context(nc.psum_tensor(f"ps{i}", [128, 512], F32)) for i in range(8)]
        psb = [p[:].bitcast(BF16) for p in ps]

        cf = A0.alloc([128, NCONST], F32)
        cb = A0.alloc([128, NCONST], BF16)
        identf = cf[:, 0:128]
        identb = cb[:, 0:128]
        maskF = cb[:, 128:256]
        maskB = cb[:, 256:384]
        indf = cf[:, 384:512]
        indb = cb[:, 384:512]
        mresb = cf[:, 512:1024]
        lball = A0.alloc([128, 16], F32)
        omlall = A0.alloc([128, 16], F32)
        ngcol = A0.alloc([128, 4], F32)
        small = A0.alloc([128, 64], F32)
        junk = A0.alloc([128, 1024], BF16)
        gates = A0.alloc([128, NT, NEXP], F32)
        hT = A0.alloc([128, 8, NTOK], BF16)
        base_regions = A0.off

        def eng_op(eng, name, R, W, **kw):
            S.op(eng, lambda e: getattr(e, name)(**kw), R, W)

        def ACT(out, in_, func, R, W, **kw):
            S.op('act', lambda e: e.activation(out=out, in_=in_, func=func, **kw), R, W)

        def MM(out, lhsT, rhs, R, W, start=True, stop=True, tp=None):
            if tp is None:
                S.op('pe', lambda e: e.matmul(out, lhsT=lhsT, rhs=rhs, start=start, stop=stop), R, W)
            else:
                S.op('pe', lambda e: e.matmul(out, lhsT=lhsT, rhs=rhs, start=start, stop=stop, tile_position=tp), R, W)

        def TR(out, in_, ident, R, W):
            S.op('pe', lambda e: e.transpose(out=out, in_=in_, identity=ident), R, W)

        def TT(eng, out, in0, in1, op, R, W):
            S.op(eng, lambda e: e.tensor_tensor(out=out, in0=in0, in1=in1, op=op), R, W)

        def TS(eng, out, in0, s1, s2, op0, op1, R, W):
            if s2 is None:
                S.op(eng, lambda e: e.tensor_scalar(out=out, in0=in0, scalar1=s1, scalar2=None, op0=op0), R, W)
            else:
                S.op(eng, lambda e: e.tensor_scalar(out=out, in0=in0, scalar1=s1, scalar2=s2, op0=op0, op1=op1), R, W)

        def STT(out, in0, scalar, in1, op0, op1, R, W):
            S.op('dve', lambda e: e.scalar_tensor_tensor(out=out, in0=in0, scalar=scalar, in1=in1, op0=op0, op1=op1), R, W)

        def DMA(q, out, in_, R, W, nc_ok=False):
            if nc_ok:
                S.dma(q, lambda e: e.dma_start(out=out, in_=in_, allow_slow_non_contiguous=True), R, W)
            else:
                S.dma(q, lambda e: e.dma_start(out=out, in_=in_), R, W)

        def rstd(ss, key):
            ACT(ss, ss, AF.Ln, [key], [key], bias=EPS)
            ACT(ss, ss, AF.Exp, [key], [key], scale=-0.5)

        DMA('sp', cf[:], cst, [], ['cf'])
        S.op('dve', lambda e: e.tensor_copy(out=cb[:], in_=cf[:]), ['cf'], ['cb'])
        CONST = ['cf', 'cb']
        DMA('sp', ngcol[:, 0:2], gla_ng.rearrange("l p -> p l"), [], ['ngcol'], nc_ok=True)
        DMA('sp', ngcol[:, 2:4], hg_ng.rearrange("l p -> p l"), [], ['ngcol'], nc_ok=True)
        DMA('sp', lball[:], hgrn_lb.rearrange("l d (h p) -> p (l d h)", p=128), [], ['lball'], nc_ok=True)
        e01 = small[:, 0:16]
        ACT(e01, lball[:], AF.Exp, ['lball'], ['e01'])
        ssum = small[:, 16:24]
        TT('dve', ssum, small[:, 0:8], small[:, 8:16], ALU.add, ['e01'], ['ssum'])
        S.op('dve', lambda e: e.reciprocal(out=ssum, in_=ssum), ['ssum'], ['ssum'])
        p0 = small[:, 24:32]
        p1 = small[:, 32:40]
        TT('dve', p0, small[:, 0:8], ssum, ALU.mult, ['e01', 'ssum'], ['p0'])
        TT('dve', p1, small[:, 8:16], ssum, ALU.mult, ['e01', 'ssum'], ['p1'])
        cum1 = small[:, 40:48]
        TT('dve', cum1, p0, p1, ALU.add, ['p0', 'p1'], ['cum1'])
        TT('dve', lball[:, 0:8], p0, p0, ALU.subtract, ['p0'], ['lball'])
        TT('dve', lball[:, 8:16], cum1, p0, ALU.subtract, ['cum1', 'p0'], ['lball'])
        TS('dve', lball[:], lball[:], 0.0, None, ALU.max, None, ['lball'], ['lball'])
        TS('dve', omlall[:], lball[:], -1.0, 1.0, ALU.mult, ALU.add, ['lball'], ['omlall'])

        Ap = Arena(nc, base_regions, LIMIT)
        ct = Ap.alloc([128, 24], F32)
        cs = Ap.alloc([128, 24], BF16)
        modv = Ap.alloc([3, 6 * D], F32)
        bm3 = Ap.alloc([3, 6 * D], F32)
        g13 = Ap.alloc([3, D], F32)
        g23 = Ap.alloc([3, D], F32)
        wm = [Ap.alloc([128, 8, 512], BF16) for _ in range(2)]
        DMA('sp', ct[:], c3T, [], ['ct'])
        ACT(cs[:], ct[:], AF.Silu, ['ct'], ['cs'])
        for l in range(n_layers):
            DMA('sp', bm3[:], b_mod[l, :].partition_broadcast(3), [], ['bm3'])
            DMA('sp', g13[:], norm1_g[l, :].partition_broadcast(3), [], ['g13'])
            DMA('sp', g23[:], norm2_g[l, :].partition_broadcast(3), [], ['g23'])
            for n in range(12):
                sl = n % 2
                DMA('pool', wm[sl][:], w_mod[l].rearrange("(k p) n -> p k n", p=128)[:, :, n * 512:(n + 1) * 512],
                    [], [f'wm{sl}'])
                for k in range(8):
                    MM(ps[sl][0:3, :], cs[:, 3 * k:3 * k + 3], wm[sl][:, k, :], ['cs', f'wm{sl}'], [f'ps{sl}'],
                       start=(k == 0), stop=(k == 7))
                TT('dve', modv[:, n * 512:(n + 1) * 512], ps[sl][0:3, :], bm3[:, n * 512:(n + 1) * 512], ALU.add,
                   [f'ps{sl}', 'bm3'], ['modv'])
            STT(modv[:, D:2 * D], modv[:, D:2 * D], 1.0, g13[:], ALU.add, ALU.mult, ['modv', 'g13'], ['modv'])
            STT(modv[:, 4 * D:5 * D], modv[:, 4 * D:5 * D], 1.0, g23[:], ALU.add, ALU.mult, ['modv', 'g23'], ['modv'])
            DMA('sp', modsc[l], modv[:], ['modv'], [f'modsc{l}'])
        S.barrier(lambda e: e.memset(small[0:1, 60:61], 0.0))

        def modvec(l, row, j):
            return modsc[l, row, j * D:(j + 1) * D].partition_broadcast(128)

        RA = base_regions
        acc = nc.alloc_sbuf_tensor_at("acc", [128, NT, D], F32, offset=RA)
        RB = RA + NT * D * 4
        RB_SIZE = 57344
        RC = RB + RB_SIZE

        def x_src(s, l, t):
            if l == 0:
                return ctx2[s, t * 128:(t + 1) * 128, :] if t < 2 else x2[s, (t - 2) * 128:(t - 1) * 128, :]
            return None

        def tile_cols(t):
            return slice(t * 128, (t + 1) * 128)

        for s in range(nseq):
            for l in range(n_layers):
                last = (l == n_layers - 1) and (l == 1)
                Ac = Arena(nc, RC, LIMIT)
                bcG = Ac.alloc([128, D], F32)
                bcS = Ac.alloc([128, D], F32)
                xt = [Ac.alloc([128, D], F32) for _ in range(2)]
                hfs = [Ac.alloc([128, D], F32) for _ in range(2)]
                ssq = Ac.alloc([128, 2], F32)
                for t in range(NT):
                    if t == 0 or t == 2:
                        row = 2 if t == 0 else s
                        DMA('sp', bcS[:], modvec(l, row, 0), [f'modsc{l}'], ['bcS'])
                        DMA('sp', bcG[:], modvec(l, row, 1), [f'modsc{l}'], ['bcG'])
                    if l == 0:
                        xa = xt[t % 2]
                        xk = f'xt{t % 2}'
                        DMA('sp', xa[:], x_src(s, l, t), [], [xk])
                        xin = xa[:]
                    else:
                        xin = acc[:, t, :]
                        xk = f'acc{t}'
                        if t >= 2:
                            DMA('sp', xs[(t - 2) * 128:(t - 1) * 128, :], acc[:, t, :], [xk], [f'xs{t}'])
                    hf = hfs[t % 2]
                    hfk = f'hf{t % 2}'
                    sq = ssq[:, t % 2:t % 2 + 1]
                    sqk = f'ssq{t % 2}'
                    ACT(junk[:], xin, AF.Square, [xk], [sqk], scale=D ** -0.5, accum_out=sq)
                    rstd(sq, sqk)
                    STT(hf[:], xin, sq, bcG[:], ALU.mult, ALU.mult, [xk, sqk, 'bcG'], [hfk])
                    TT('pool', hf[:], hf[:], bcS[:], ALU.add, [hfk, 'bcS'], [hfk])
                    for k in range(8):
                        TR(ps[(k // 4)][:, (k % 4) * 128:(k % 4 + 1) * 128], hf[:, k * 128:(k + 1) * 128], identf,
                           [hfk] + CONST, [f'ps{k // 4}'])
                    for hh in range(2):
                        ACT(hT[:, 4 * hh:4 * hh + 4, tile_cols(t)], ps[hh][:].rearrange("p (k c) -> p k c", c=128), AF.Copy,
                            [f'ps{hh}'], [f'hT{t}'])
                S.barrier(lambda e: e.memset(small[0:1, 60:61], 0.0))

                mixT = nc.alloc_sbuf_tensor_at(f"mixT_{s}_{l}", [128, 8, NTOK], BF16, offset=RB)
                wout = nc.alloc_sbuf_tensor_at(f"wout_{s}_{l}", [128, 8, D], BF16, offset=RB + 8 * NTOK * 2)
                Aa = Arena(nc, RA, RB)
                Ac = Arena(nc, RC, LIMIT)
                sgT = Aa.alloc([128, NTOK], BF16)
                qtT = [Aa.alloc([128, NTOK], BF16) for _ in range(2)]
                ktT = [Aa.alloc([128, NTOK], BF16) for _ in range(2)]
                kdec = [Aa.alloc([128, NT, 128], BF16) for _ in range(2)]
                vbf = Aa.alloc([128, NT, 128], BF16)
                o_fb = [Aa.alloc([128, NT, 128], F32) for _ in range(2)]
                a_all = [Aa.alloc([128, NT * NCH], F32) for _ in range(2)]
                win = [Aa.alloc([128, 8, 640], BF16)]
                w2a = [Aa.alloc([32, 256], BF16) for _ in range(2)]
                Sbf = [Aa.alloc([128, NCH, 128], BF16) for _ in range(2)]
                vblk = [Aa.alloc([128, NCH, 128], BF16) for _ in range(2)]
                PT = [Aa.alloc([128, 128], BF16) for _ in range(2)]
                tmpP = nc.alloc_sbuf_tensor_at(f"tmpP_{s}_{l}", [128, LAT], BF16, offset=Ac.off)
                tmpP2 = nc.alloc_sbuf_tensor_at(f"tmpP2_{s}_{l}", [128, LAT], BF16, offset=Ac.off + 4096)
                qf = Ac.alloc([128, 512], F32)
                kf = [Ac.alloc([128, 512], F32) for _ in range(2)]
                uu = [Ac.alloc([128, 512], F32) for _ in range(2)]
                bF = Ac.alloc([128, 512], F32)
                bb = Ac.alloc([128, 512], F32)
                ee = Ac.alloc([128, 512], F32)
                rr = ee
                e2 = Ac.alloc([128, 512], F32)
                t1 = Ac.alloc([128, 512], F32)
                vTb = Ac.alloc([128, 512], BF16)
                kdT = Ac.alloc([128, 512], BF16)
                otot = [Ac.alloc([128, 128], F32) for _ in range(2)]
                onb = [Ac.alloc([128, 128], BF16) for _ in range(2)]
                wgr = Aa.alloc([128, 8, 32], BF16)
                Sst = [[Aa.alloc([128, 128], F32) for _ in range(2)], [Ac.alloc([128, 128], F32) for _ in range(2)]]
                oss = Ac.alloc([128, 2], F32)
                gfa = [Ac.alloc([32, NTOK], BF16) for _ in range(2)]

                DMA('pool', wout[:], w_out[l].rearrange("(k p) n -> p k n", p=128), [], ['wout'])
                winl = w_in[l].rearrange("(k p) n -> p k n", p=128)
                DMA('pool', wgr[:], winl[:, :, 1536:1568], [], ['wgr'])
                for d in range(2):
                    eng_op('pool', 'memset', [], [f'gfa{d}'], ap=gfa[d][:], constant=1.0)
                    DMA('pool', w2a[d][0:16, :], gate_w2[l, d], [], [f'w2a{d}'])
                    DMA('pool', w2a[d][16:17, :], gate_b[l, d:d + 1, :], [], [f'w2a{d}'])
                for bi, (b0, bn) in enumerate(BLKS):
                    for d in range(2):
                        for k in range(8):
                            MM(ps[d][0:16, 0:bn], wgr[:, k, 16 * d:16 * d + 16], hT[:, k, b0:b0 + bn],
                               ['wgr'] + [f'hT{t}' for t in range(b0 // 128, (b0 + bn) // 128)], [f'ps{d}'],
                               start=(k == 0), stop=(k == 7))
                        ACT(gfa[d][0:16, b0:b0 + bn], ps[d][0:16, 0:bn], AF.Copy, [f'ps{d}'], [f'gfa{d}'])

                def hblk(k, cm, bi):
                    b0, bn = BLKS[bi]
                    return hT[:, k, b0:b0 + bn]

                ALLH = [f'hT{t}' for t in range(NT)]
                for hd in range(8):
                    gla = hd < 4
                    if hd == 4:
                        LATK = [f'hT{t}' for t in range(2, NT)]
                        for k in range(8):
                            tp_, tk_ = (tmpP, ['qf', 'kf0']) if k % 2 == 0 else (tmpP2, ['kf1', 'uu0'])
                            ACT(tp_[:].rearrange("p (w r) -> p w r", w=64), hT[:, k, 256:NTOK].rearrange("p (r w) -> p w r", w=64), AF.Copy,
                                LATK, tk_)
                            eng_op('dve', 'tensor_copy', tk_, LATK, out=hT[:, k, 256:NTOK].bitcast(F32), in_=tp_[:].bitcast(F32))
                    h = hd % 4
                    dk = 64 if gla else 128
                    gs = -1.0 / 16 if gla else -1.0
                    wslot = 0
                    wt = win[wslot]
                    wk = f'win{wslot}'
                    def emit_win(hx):
                        hh_ = hx % 4
                        if hx < 4:
                            pieces = [(64 * hh_, 64, 0), (256 + 64 * hh_, 64, 64), (512 + 128 * hh_, 128, 128), (1024 + 128 * hh_, 128, 256)]
                        else:
                            pieces = [(1568 + 128 * hh_, 128, 0), (2080 + 128 * hh_, 128, 128), (2592 + 128 * hh_, 128, 256),
                                      (3104 + 128 * hh_, 128, 384), (3616 + 128 * hh_, 128, 512)]
                        for (c0, w, o) in pieces:
                            DMA('pool', win[0][:, :, o:o + w], winl[:, :, c0:c0 + w], [], ['win0'])
                    if hd == 0:
                        emit_win(0)
                    ngc = ngcol[:, l:l + 1] if gla else ngcol[:, 2 + l:3 + l]

                    for bi, (b0, bn) in enumerate(BLKS):
                        t0 = b0 // 128
                        ntl = bn // 128

                        def emit_proj(bj):
                            pb0, pbn = BLKS[bj]

                            def proj(bank, M, co):
                                for k in range(8):
                                    MM(ps[bank][0:M, 0:pbn], wt[:, k, co:co + M], hblk(k, not gla, bj), [wk] + ALLH, [f'ps{bank}'],
                                       start=(k == 0), stop=(k == 7))
                            if gla:
                                proj(0, 64, 0)
                                proj(1, 64, 64)
                                proj(2, 128, 128)
                                proj(3, 128, 256)
                                for d in range(2):
                                    MM(ps[4 + d][0:64, 0:pbn], w2a[d][0:17, 64 * h:64 * h + 64], gfa[d][0:17, pb0:pb0 + pbn],
                                       [f'w2a{d}', f'gfa{d}'], [f'ps{4 + d}'])
                            else:
                                proj(0, 128, 0)
                                proj(1, 128, 128)
                                proj(4, 128, 256)
                                proj(2, 128, 384)
                                proj(3, 128, 512)
                        if bi == 0:
                            emit_proj(0)
                        if gla:
                            TS('dve', qf[0:64, 0:bn], ps[0][0:64, 0:bn], 0.125, None, ALU.mult, None, ['ps0'], ['qf'])
                            eng_op('dve', 'tensor_copy', ['ps1'], ['kf0'], out=kf[0][0:64, 0:bn], in_=ps[1][0:64, 0:bn])
                            kfd = [kf[0], kf[0]]
                            kfk = ['kf0', 'kf0']
                            for d in range(2):
                                ACT(ee[0:64, 0:bn], ps[4 + d][0:64, 0:bn], AF.Exp, [f'ps{4 + d}'], ['ee'], scale=-1.0)
                                ACT(uu[d][0:64, 0:bn], ee[0:64, 0:bn], AF.Ln, ['ee'], [f'uu{d}'], bias=1.0)
                        else:
                            ACT(qf[:, 0:bn], ps[0][:, 0:bn], AF.Silu, ['ps0'], ['qf'])
                            kfd = kf
                            kfk = ['kf0', 'kf1']
                            for d in range(2):
                                bank = 1 if d == 0 else 4
                                col = l * 8 + d * 4 + h
                                ACT(ee[:, 0:bn], ps[bank][:, 0:bn], AF.Exp, [f'ps{bank}'], ['ee'], scale=-1.0)
                                ACT(bb[:, 0:bn], ee[:, 0:bn], AF.Ln, ['ee'], ['bb'], bias=1.0)
                                ACT(t1[:, 0:bn], ee[:, 0:bn], AF.Ln, ['ee', 'lball'], ['t1'], bias=1.0, scale=lball[:, col:col + 1])
                                TT('pool', uu[d][:, 0:bn], bb[:, 0:bn], t1[:, 0:bn], ALU.subtract, ['bb', 't1'], [f'uu{d}'])
                                ACT(e2[:, 0:bn], bb[:, 0:bn], AF.Exp, ['bb'], ['e2'], scale=-1.0)
                                STT(kf[d][:, 0:bn], ee[:, 0:bn], omlall[:, col:col + 1], e2[:, 0:bn], ALU.mult, ALU.mult,
                                    ['ee', 'e2', 'omlall'], [f'kf{d}'])
                        ACT(vTb[:, 0:bn], ps[2][:, 0:bn], AF.Copy, ['ps2'], ['vTb'])
                        ACT(sgT[:, b0:b0 + bn], ps[3][:, 0:bn], AF.Silu, ['ps3'], [f'sgT{bi}'])
                        if bi + 1 < len(BLKS):
                            emit_proj(bi + 1)
                        for j in range(ntl):
                            TR(psb[6][:, j * 128:(j + 1) * 128], vTb[:, j * 128:(j + 1) * 128], identb, ['vTb'] + CONST, ['ps6'])
                        eng_op('dve', 'tensor_copy', ['ps6'], [f'vbf{t}' for t in range(t0, t0 + ntl)],
                               out=vbf[:, t0:t0 + ntl, :], in_=psb[6][:, 0:bn].rearrange("p (j c) -> p j c", c=128))
                        for d in range(2):
                            nchb = bn // C
                            eng_op('dve', 'tensor_tensor_scan', [f'uu{d}'] + CONST, ['bF'], out=bF[0:dk, 0:bn], data0=mresb[0:dk, 0:bn],
                                   data1=uu[d][0:dk, 0:bn], initial=0.0, op0=ALU.mult, op1=ALU.add)
                            blast = bF[0:dk, 0:bn].rearrange("p (c j) -> p c j", j=C)[:, :, C - 1:C]
                            bl_bc = blast.to_broadcast([dk, nchb, C])
                            bF3 = bF[0:dk, 0:bn].rearrange("p (c j) -> p c j", j=C)
                            rr3 = rr[0:dk, 0:bn].rearrange("p (c j) -> p c j", j=C)
                            if d == 0:
                                bsrc = bF
                                bkey = 'bF'
                                TT('dve', rr3, bl_bc, bF3, ALU.subtract, ['bF'], ['ee'])
                            else:
                                TT('dve', rr[0:dk, 0:bn], bF[0:dk, 0:bn], uu[d][0:dk, 0:bn], ALU.subtract, ['bF', f'uu{d}'], ['ee'])
                                bb3 = bb[0:dk, 0:bn].rearrange("p (c j) -> p c j", j=C)
                                TT('pool', bb3, bl_bc, rr3, ALU.subtract, ['bF', 'ee'], ['bb'])
                                bsrc = bb
                                bkey = 'bb'
                            ACT(a_all[d][0:dk, (b0 // C):(b0 // C) + nchb], bF[0:dk, 0:bn].rearrange("p (c j) -> p c j", j=C)[:, :, C - 1],
                                AF.Exp, ['bF'], [f'a{d}'], scale=gs)
                            ACT(e2[0:dk, 0:bn], bsrc[0:dk, 0:bn], AF.Exp, [bkey], ['e2'], scale=gs)
                            TT('dve', qtT[d][0:dk, b0:b0 + bn], qf[0:dk, 0:bn], e2[0:dk, 0:bn], ALU.mult, ['qf', 'e2'], [f'qtT{d}_{bi}'])
                            ACT(t1[0:dk, 0:bn], bsrc[0:dk, 0:bn], AF.Exp, [bkey], ['t1'], scale=-gs)
                            TT('dve', ktT[d][0:dk, b0:b0 + bn], kfd[d][0:dk, 0:bn], t1[0:dk, 0:bn], ALU.mult, [kfk[d], 't1'], [f'ktT{d}_{bi}'])
                            ACT(e2[0:dk, 0:bn], rr[0:dk, 0:bn], AF.Exp, ['ee'], ['e2'], scale=gs)
                            TT('pool', kdT[0:dk, 0:bn], kfd[d][0:dk, 0:bn], e2[0:dk, 0:bn], ALU.mult, [kfk[d], 'e2'], ['kdT'])
                            for j in range(ntl):
                                TR(psb[7][:, j * 128:j * 128 + dk], kdT[0:dk, j * 128:(j + 1) * 128], identb[0:dk, 0:dk], ['kdT'] + CONST, ['ps7'])
                            ACT(kdec[d][:, t0:t0 + ntl, 0:dk], psb[7][:, 0:bn].rearrange("p (j c) -> p j c", c=128)[:, :, 0:dk], AF.Copy,
                                ['ps7'], [f'kdec{d}_{t}' for t in range(t0, t0 + ntl)])

                    if hd + 1 < 8:
                        emit_win(hd + 1)
                    orders = [list(range(NT)), [1, 0] + list(range(NT - 1, 1, -1))]
                    corders = [list(range(NCH)), list(range(NCH - 1, -1, -1))]
                    masks = [maskF, maskB]
                    PKV = [5, 6]
                    PSC = [0, 3]
                    PO = [1, 4]
                    for d in range(2):
                        eng_op('pool', 'memset', [], [f'Sst{d}_0'], ap=Sst[d][0][:], constant=0.0)
                        eng_op('pool', 'memset', [], [f'Sbf{d}_0'], ap=Sbf[d][:, 0, :], constant=0.0)
                    sp = [0, 0]
                    for it in range(NT):
                        tt = [orders[d][it] for d in range(2)]
                        need = [not (last and tt[d] < 2) for d in range(2)]
                        bis = [0 if tt[d] < 2 else 1 + (tt[d] - 2) // 4 for d in range(2)]
                        for d in range(2):
                            t = tt[d]
                            TT('dve', vblk[d][:], vbf[:, t, :].unsqueeze(1).to_broadcast([128, NCH, 128]),
                               indf[:, 0:NCH].unsqueeze(2).to_broadcast([128, NCH, 128]), ALU.mult, [f'vbf{t}'] + CONST, [f'vblk{d}'])
                            MM(ps[PKV[d]][0:dk, :], kdec[d][:, t, 0:dk], vblk[d][:].rearrange("p c e -> p (c e)"), [f'kdec{d}_{t}', f'vblk{d}'], [f'ps{PKV[d]}'])
                        for d in range(2):
                            t = tt[d]
                            if need[d]:
                                MM(ps[PSC[d]][:, 0:128], ktT[d][0:dk, tile_cols(t)], qtT[d][0:dk, tile_cols(t)],
                                   [f'ktT{d}_{bis[d]}', f'qtT{d}_{bis[d]}'], [f'ps{PSC[d]}'])
                        for d in range(2):
                            t = tt[d]
                            if need[d]:
                                TT('dve', PT[d][:], ps[PSC[d]][:, 0:128], masks[d], ALU.mult, [f'ps{PSC[d]}'] + CONST, [f'PT{d}'])
                                MM(ps[PO[d]][:, 0:128], PT[d][:], vbf[:, t, :], [f'PT{d}', f'vbf{t}'], [f'ps{PO[d]}'], start=True, stop=False)
                        for ci in range(NCH):
                            for d in range(2):
                                t = tt[d]
                                c = corders[d][ci]
                                if need[d]:
                                    MM(ps[PO[d]][C * c:C * c + C, 0:128], qtT[d][0:dk, t * 128 + C * c:t * 128 + C * c + C], Sbf[d][0:dk, ci, :],
                                       [f'qtT{d}_{bis[d]}', f'Sbf{d}_{ci}'], [f'ps{PO[d]}'], start=False, stop=True, tp=(0, C * c))
                            for d in range(2):
                                t = tt[d]
                                c = corders[d][ci]
                                p = sp[d]
                                STT(Sst[d][1 - p][0:dk, :], Sst[d][p][0:dk, :], a_all[d][0:dk, t * NCH + c:t * NCH + c + 1],
                                    ps[PKV[d]][0:dk, c * 128:(c + 1) * 128], ALU.mult, ALU.add, [f'Sst{d}_{p}', f'a{d}', f'ps{PKV[d]}'], [f'Sst{d}_{1 - p}'])
                                sp[d] = 1 - p
                            for d in range(2):
                                nslot = (ci + 1) if ci < NCH - 1 else 0
                                ACT(Sbf[d][0:dk, nslot, :], Sst[d][sp[d]][0:dk, :], AF.Copy, [f'Sst{d}_{sp[d]}'], [f'Sbf{d}_{nslot}'])
                        for d in range(2):
                            if need[d]:
                                ACT(o_fb[d][:, tt[d], :], ps[PO[d]][:, 0:128], AF.Copy, [f'ps{PO[d]}'], [f'o{d}_{tt[d]}'])
                    for t in range(NT):
                        if last and t < 2:
                            continue
                        bi = 0 if t < 2 else 1 + (t - 2) // 4
                        es = t % 2
                        epb = 2 if es == 0 else 7
                        TT('pool', otot[es][:], o_fb[0][:, t, :], o_fb[1][:, t, :], ALU.add, [f'o0_{t}', f'o1_{t}'], [f'otot{es}'])
                        ACT(junk[:, 0:128], otot[es][:], AF.Square, [f'otot{es}'], [f'oss{es}'], scale=128 ** -0.5, accum_out=oss[:, es:es + 1])
                        rstd(oss[:, es:es + 1], f'oss{es}')
                        TS('dve', onb[es][:], otot[es][:], oss[:, es:es + 1], None, ALU.mult, None, [f'otot{es}', f'oss{es}'], [f'onb{es}'])
                        TR(psb[epb][:, 0:128], onb[es][:], identb, [f'onb{es}'] + CONST, [f'ps{epb}'])
                        if gla or t < 2:
                            STT(mixT[:, hd, tile_cols(t)], psb[epb][:, 0:128], ngc, sgT[:, tile_cols(t)], ALU.mult, ALU.mult,
                                [f'ps{epb}', f'sgT{bi}', 'ngcol'], [f'mixT{hd}'])
                        else:
                            j = t - 2
                            ov = mixT[:, hd, 256:NTOK].rearrange("p (r w) -> p w r", w=64)[:, 4 * j:4 * j + 4, :]
                            STT(ov, psb[epb][:, 0:128].rearrange("p (w r) -> p w r", w=4), ngc,
                                sgT[:, tile_cols(t)].rearrange("p (w r) -> p w r", w=4), ALU.mult, ALU.mult,
                                [f'ps{epb}', f'sgT{bi}', 'ngcol'], [f'mixT{hd}'])
                S.barrier(lambda e: e.memset(small[0:1, 60:61], 0.0))

                Ac = Arena(nc, RC, LIMIT)
                bcG1 = Ac.alloc([128, D], F32)
                bcG2 = Ac.alloc([128, D], F32)
                bcS2 = Ac.alloc([128, D], F32)
                xt = [Ac.alloc([128, D], F32) for _ in range(2)]
                Bb = [Ac.alloc([128, D], F32) for _ in range(2)]
                hTf = Ac.alloc([128, 8, 128], F32)
                rtf = Ac.alloc([128, 8, NEXP], F32)
                lg = Ac.alloc([128, 4 * NEXP], F32)
                ssq = Ac.alloc([128, 8], F32)
                moe = (l % 2 == 1)
                if moe:
                    DMA('sp', rtf[:], router[0].rearrange("(k p) e -> p k e", p=128), [], ['rtf'], nc_ok=True)
                MIXK = [f'mixT{hd}' for hd in range(8)]
                for t in range(NT):
                    if last and t < 2:
                        continue
                    if t == 0 or t == 2:
                        row = 2 if t == 0 else s
                        DMA('sp', bcG1[:], modvec(l, row, 2), [f'modsc{l}'], ['bcG1'])
                        DMA('sp', bcS2[:], modvec(l, row, 3), [f'modsc{l}'], ['bcS2'])
                        DMA('sp', bcG2[:], modvec(l, row, 4), [f'modsc{l}'], ['bcG2'])
                    tmp = hf = Bb[t % 2]
                    bk = f'B{t % 2}'
                    sq = ssq[:, 7 * (t % 2):7 * (t % 2) + 1]
                    sqk = f'ssqn{t % 2}'
                    xa = xt[t % 2]
                    xk = f'xt{t % 2}'
                    if l == 0:
                        DMA('sp', xa[:], x_src(s, l, t), [], [xk])
                    else:
                        DMA('sp', xa[:], xs[(t - 2) * 128:(t - 1) * 128, :], [f'xs{t}'], [xk])
                    for nh in range(2):
                        for k in range(8):
                            MM(ps[nh][:, :], mixT[:, k, tile_cols(t)], wout[:, k, nh * 512:(nh + 1) * 512], MIXK + ['wout'], [f'ps{nh}'],
                               start=(k == 0), stop=(k == 7))
                        TT('dve', tmp[:, nh * 512:(nh + 1) * 512], ps[nh][:, :], bcG1[:, nh * 512:(nh + 1) * 512], ALU.mult,
                           [f'ps{nh}', 'bcG1'], [bk])
                    TT('pool', acc[:, t, :], xa[:], tmp[:], ALU.add, [xk, bk], [f'acc{t}'])
                    ak = f'acc{t}'
                    ACT(junk[:], acc[:, t, :], AF.Square, [ak], [sqk], scale=D ** -0.5, accum_out=sq)
                    rstd(sq, sqk)
                    STT(hf[:], acc[:, t, :], sq, bcG2[:], ALU.mult, ALU.mult, [ak, sqk, 'bcG2', bk], [bk])
                    TT('pool', hf[:], hf[:], bcS2[:], ALU.add, [bk, 'bcS2'], [bk])
                    for k in range(8):
                        TR(ps[2 + (k // 4)][:, (k % 4) * 128:(k % 4 + 1) * 128], hf[:, k * 128:(k + 1) * 128], identf,
                           [bk] + CONST, [f'ps{2 + k // 4}'])
                    for hh in range(2):
                        ACT(hT[:, 4 * hh:4 * hh + 4, tile_cols(t)], ps[2 + hh][:].rearrange("p (k c) -> p k c", c=128), AF.Copy,
                            [f'ps{2 + hh}'], [f'hT{t}'])
                    if moe and t >= 2 and not DBG_NOROUTER:
                        for hh in range(2):
                            ACT(hTf[:, 4 * hh:4 * hh + 4, :], ps[2 + hh][:].rearrange("p (k c) -> p k c", c=128), AF.Copy, [f'ps{2 + hh}'], ['hTf'])
                        if DBG_SUB == '1':
                            continue
                        for k in range(8):
                            MM(ps[4][:, 0:NEXP], hTf[:, k, :], rtf[:, k, :], ['hTf', 'rtf'], ['ps4'], start=(k == 0), stop=(k == 7))
                        if DBG_SUB == '2':
                            continue
                        L = lg[:, 0:8]
                        EQ1 = lg[:, 8:16]
                        L2 = lg[:, 16:24]
                        EQ2 = lg[:, 24:32]
                        eng_op('dve', 'tensor_copy', ['ps4'], ['lg'], out=L, in_=ps[4][:, 0:NEXP])
                        eng_op('dve', 'reduce_max', ['lg'], ['ssq'], out=ssq[:, 1:2], in_=L, axis=mybir.AxisListType.X)
                        TS('dve', EQ1, L, ssq[:, 1:2], None, ALU.is_equal, None, ['lg', 'ssq'], ['lg'])
                        STT(L2, EQ1, -1e30, L, ALU.mult, ALU.add, ['lg'], ['lg'])
                        eng_op('dve', 'reduce_max', ['lg'], ['ssq'], out=ssq[:, 2:3], in_=L2, axis=mybir.AxisListType.X)
                        TS('dve', EQ2, L2, ssq[:, 2:3], None, ALU.is_equal, None, ['lg', 'ssq'], ['lg'])
                        if DBG_SUB == '3':
                            continue
                        TT('dve', ssq[:, 3:4], ssq[:, 2:3], ssq[:, 1:2], ALU.subtract, ['ssq'], ['ssq'])
                        ACT(ssq[:, 4:5], ssq[:, 3:4], AF.Exp, ['ssq'], ['ssq'])
                        TS('dve', ssq[:, 5:6], ssq[:, 4:5], 1.0, None, ALU.add, None, ['ssq'], ['ssq'])
                        eng_op('dve', 'reciprocal', ['ssq'], ['ssq'], out=ssq[:, 5:6], in_=ssq[:, 5:6])
                        TT('dve', ssq[:, 6:7], ssq[:, 4:5], ssq[:, 5:6], ALU.mult, ['ssq'], ['ssq'])
                        TS('dve', gates[:, t, :], EQ1, ssq[:, 5:6], None, ALU.mult, None, ['lg', 'ssq'], [f'gates{t}'])
                        STT(gates[:, t, :], EQ2, ssq[:, 6:7], gates[:, t, :], ALU.mult, ALU.add, ['lg', 'ssq', f'gates{t}'], [f'gates{t}'])
                if stop == ('mix', l):
                    for t in range(NT):
                        DMA('sp', dbg[s, t * 128:(t + 1) * 128, :], acc[:, t, :], [f'acc{t}'], [f'dbg{s}_{t}'])
                    S.barrier(lambda e: e.memset(small[0:1, 60:61], 0.0))
                    break
                S.barrier(lambda e: e.memset(small[0:1, 60:61], 0.0))

                Ab = Arena(nc, RB, RC)
                GS = 4 if moe else 2
                w1g = [Ab.alloc([128, 8, GS * 128], BF16) for _ in range(2)]
                w3g = [Ab.alloc([128, 8, GS * 128], BF16) for _ in range(2)]
                w2g = [Ab.alloc([128, GS, D], BF16) for _ in range(2)]
                aT = [Ab.alloc([128, GS, 512], BF16) for _ in range(2)]
                Ac2 = Arena(nc, RC, LIMIT)
                s1 = [Ac2.alloc([128, 512], F32) for _ in range(2)]
                bcg = [Ac2.alloc([128, D], F32) for _ in range(2)]
                tm = [Ac2.alloc([128, D], F32) for _ in range(2)]
                DMA('sp', bcg[0][:], modvec(l, 2, 5), [f'modsc{l}'], ['bcg0'])
                DMA('sp', bcg[1][:], modvec(l, s, 5), [f'modsc{l}'], ['bcg1'])
                nE = NEXP if moe else 1
                FF = DEXP if moe else DFF
                ngroups = FF // (GS * 128)
                tblocks = ([] if last else [(0, 256)]) + [(256 + 512 * j, 512) for j in range(4)]
                groups = [(ex, g) for ex in range(nE) for g in range(ngroups)]

                def emit_wdma(gidx):
                    ex, g = groups[gidx]
                    sl = gidx % 2
                    W1 = moe_w1[0, ex] if moe else ffn_w1[0]
                    W3 = moe_w3[0, ex] if moe else ffn_w3[0]
                    W2 = moe_w2[0, ex] if moe else ffn_w2[0]
                    f0 = g * GS * 128
                    DMA('pool', w1g[sl][:], W1.rearrange("(k p) n -> p k n", p=128)[:, :, f0:f0 + GS * 128], [], [f'w1g{sl}'])
                    DMA('pool', w3g[sl][:], W3.rearrange("(k p) n -> p k n", p=128)[:, :, f0:f0 + GS * 128], [], [f'w3g{sl}'])
                    DMA('pool', w2g[sl][:], W2[f0:f0 + GS * 128, :].rearrange("(g p) n -> p g n", p=128), [], [f'w2g{sl}'])

                items = [(gidx, bidx) for gidx in range(len(groups)) for bidx in range(len(tblocks))]

                def stage1(ii):
                    gidx, bidx = items[ii]
                    sl = gidx % 2
                    asl = ii % 2
                    b0, bn = tblocks[bidx]
                    hk = [f'hT{t}' for t in range(b0 // 128, (b0 + bn) // 128)]
                    for f in range(GS):
                        pb = 2 * (f % 2)
                        for k in range(8):
                            MM(ps[pb][:, 0:bn], w1g[sl][:, k, f * 128:(f + 1) * 128], hT[:, k, b0:b0 + bn], [f'w1g{sl}'] + hk, [f'ps{pb}'],
                               start=(k == 0), stop=(k == 7))
                        for k in range(8):
                            MM(ps[pb + 1][:, 0:bn], w3g[sl][:, k, f * 128:(f + 1) * 128], hT[:, k, b0:b0 + bn], [f'w3g{sl}'] + hk, [f'ps{pb + 1}'],
                               start=(k == 0), stop=(k == 7))
                        ACT(s1[f % 2][:, 0:bn], ps[pb][:, 0:bn], AF.Silu, [f'ps{pb}'], [f's1{f % 2}'])
                        TT('dve', aT[asl][:, f, 0:bn], ps[pb + 1][:, 0:bn], s1[f % 2][:, 0:bn], ALU.mult,
                           [f'ps{pb + 1}', f's1{f % 2}'], [f'aT{asl}'])

                tcount = [0]

                def stage2(ii):
                    gidx, bidx = items[ii]
                    ex, g = groups[gidx]
                    sl = gidx % 2
                    asl = ii % 2
                    b0, bn = tblocks[bidx]
                    for j in range(bn // 128):
                        t = b0 // 128 + j
                        tsl = tcount[0] % 2
                        tcount[0] += 1
                        for nh in range(2):
                            pbk = 4 + 2 * tsl + nh
                            for f in range(GS):
                                MM(ps[pbk][:, :], aT[asl][:, f, j * 128:(j + 1) * 128], w2g[sl][:, f, nh * 512:(nh + 1) * 512],
                                   [f'aT{asl}', f'w2g{sl}'], [f'ps{pbk}'], start=(f == 0), stop=(f == GS - 1))
                            gsc = gates[:, t, ex:ex + 1] if moe else 1.0
                            bg = bcg[0 if t < 2 else 1]
                            STT(tm[tsl][:, nh * 512:(nh + 1) * 512], ps[pbk][:, :], gsc, bg[:, nh * 512:(nh + 1) * 512], ALU.mult, ALU.mult,
                                [f'ps{pbk}', f'gates{t}', 'bcg0', 'bcg1'], [f'tm{tsl}'])
                        TT('pool', acc[:, t, :], acc[:, t, :], tm[tsl][:], ALU.add, [f'acc{t}', f'tm{tsl}'], [f'acc{t}'])

                emit_wdma(0)
                if len(groups) > 1:
                    emit_wdma(1)
                stage1(0)
                for ii in range(len(items)):
                    if ii + 1 < len(items):
                        stage1(ii + 1)
                    stage2(ii)
                    gidx, bidx = items[ii]
                    if bidx == len(tblocks) - 1 and gidx + 2 < len(groups):
                        emit_wdma(gidx + 2)
                if stop == ('ffn', l):
                    for t in range(NT):
                        DMA('sp', dbg[s, t * 128:(t + 1) * 128, :], acc[:, t, :], [f'acc{t}'], [f'dbg{s}_{t}'])
                    S.barrier(lambda e: e.memset(small[0:1, 60:61], 0.0))
                    break
                S.barrier(lambda e: e.memset(small[0:1, 60:61], 0.0))
            else:
                Ac = Arena(nc, RC, LIMIT)
                fg = Ac.alloc([128, D], F32)
                ot = [Ac.alloc([128, D], F32) for _ in range(2)]
                ssq = Ac.alloc([128, 2], F32)
                DMA('sp', fg[:], fin_g.partition_broadcast(128), [], ['fg'])
                for t in range(2, NT):
                    ACT(junk[:], acc[:, t, :], AF.Square, [f'acc{t}'], ['ssq'], scale=D ** -0.5, accum_out=ssq[:, 0:1])
                    rstd(ssq[:, 0:1], 'ssq')
                    STT(ot[t % 2][:], acc[:, t, :], ssq[:, 0:1], fg[:], ALU.mult, ALU.mult, [f'acc{t}', 'ssq', 'fg'], [f'ot{t % 2}'])
                    DMA('sp', out2[s, (t - 2) * 128:(t - 1) * 128, :], ot[t % 2][:], [f'ot{t % 2}'], [f'out{s}_{t}'])
                S.barrier(lambda e: e.memset(small[0:1, 60:61], 0.0))
        S.op('sp', None, [k for k in S.last_w if isinstance(k, str) and (k.startswith('out') or k.startswith('dbg'))], [])
        S.emit(nc, st)
    return nc


_CACHE = {}


def make_in_maps(inputs, ncores=8):
    f = lambda a: np.ascontiguousarray(np.asarray(a, dtype=np.float32))
    cst = make_consts()
    shared = {k: f(inputs[k]) for k in ("w_mod", "b_mod", "norm1_g", "norm2_g", "w_in", "gla_gate_w2", "gla_gate_b",
                                        "gla_norm_g", "hgrn_norm_g", "hgrn_lb", "w_out", "ffn_w1", "ffn_w3", "ffn_w2",
                                        "moe_router", "moe_w1", "moe_w3", "moe_w2", "final_norm_g")}
    x, c, ctx, c_ctx = f(inputs["x"]), f(inputs["c"]), f(inputs["ctx"]), f(inputs["c_ctx"])
    maps = []
    for i in range(ncores):
        c3 = np.stack([c[2 * i], c[2 * i + 1], c_ctx], axis=0)
        c3T = np.ascontiguousarray(c3.reshape(3, 8, 128).transpose(2, 1, 0).reshape(128, 24))
        m = dict(shared)
        m.update(x2=np.ascontiguousarray(x[2 * i:2 * i + 2]), ctx2=np.ascontiguousarray(ctx[2 * i:2 * i + 2]), c3T=c3T, cst=cst)
        maps.append(m)
    return maps


def kernel(**inputs):
    if 'nc' not in _CACHE:
        _CACHE['nc'] = build()
    nc = _CACHE['nc']
    maps = make_in_maps(inputs)
    res = run_bass_kernel_spmd(nc, maps, core_ids=list(range(8)))
    return np.concatenate([r["out2"] for r in res.results], axis=0).astype(np.float32)
```
